# Optimizing a Trainium2 kernel written in Bass

```python
import jax, jax.numpy as jnp
from jax import lax
import numpy as np

D_MODEL = 4096
BATCH = 4
SEQ = 4096
DEPTH = 1

D_POOL = D_MODEL // 2
POOL_WINDOWS = (2, 4, 8, 16)
N_POOL_GROUPS = len(POOL_WINDOWS)
POOL_GROUP = D_POOL // N_POOL_GROUPS
D_SGU = D_MODEL // 2
SGU_CHUNK = 128
SGU_HEADS = 16
SGU_HEAD_DIM = D_SGU // SGU_HEADS
N_BRANCH = 2
D_IN = D_POOL + 2 * D_SGU + N_BRANCH * D_MODEL
SPLITS = (D_POOL, D_POOL + D_SGU, D_POOL + 2 * D_SGU, D_POOL + 2 * D_SGU + D_MODEL)
N_EXPERTS = 64
TOP_K = 8
N_GROUPS = 8
TOPK_GROUPS = 4
D_EXPERT = 512
D_SHARED = 512
ROUTED_SCALE = 2.5
MOE_BLOCK = 128
N_MOD = 6
EPS = 1e-6

kernel_name = "gated_pool_sgu_moe_block"


def _rmsnorm(x):
    xf = x.astype(jnp.float32)
    y = xf * lax.rsqrt(jnp.mean(xf * xf, axis=-1, keepdims=True) + EPS)
    return y.astype(x.dtype)


def _modulate(h, shift, scale):
    return h * (1.0 + scale[:, None, :]) + shift[:, None, :]


def _pool_mixer(a, w_pool, b_pool, pool_scale):
    bsz, seq, _ = a.shape
    af = a.astype(jnp.float32).reshape(bsz, seq, N_POOL_GROUPS, POOL_GROUP)
    csum = jnp.cumsum(af, axis=1)
    pos = jnp.arange(seq)
    pooled = []
    for g, w in enumerate(POOL_WINDOWS):
        cs = csum[:, :, g, :]
        lagged = jnp.pad(cs, ((0, 0), (w, 0), (0, 0)))[:, :seq, :]
        count = jnp.minimum(pos + 1, w).astype(jnp.float32)[None, :, None]
        pooled.append((cs - lagged) / count)
    pooled = jnp.stack(pooled, axis=2)
    diff = (pooled - af).astype(a.dtype)
    y = jnp.einsum('bsgc,gcd->bsgd', diff, w_pool) + b_pool
    return y.reshape(bsz, seq, D_POOL) * pool_scale


def _sgu_mixer(u, v, ln_g, ln_b, w_spatial, b_spatial):
    bsz, seq, _ = u.shape
    vf = v.astype(jnp.float32)
    mu = jnp.mean(vf, axis=-1, keepdims=True)
    var = jnp.mean(jnp.square(vf - mu), axis=-1, keepdims=True)
    vn = ((vf - mu) * lax.rsqrt(var + EPS)).astype(v.dtype) * ln_g + ln_b
    n_chunks = seq // SGU_CHUNK
    vc = vn.reshape(bsz, n_chunks, SGU_CHUNK, SGU_HEADS, SGU_HEAD_DIM)
    causal = jnp.tril(jnp.ones((SGU_CHUNK, SGU_CHUNK), dtype=bool))
    ws = jnp.where(causal[None], w_spatial, jnp.zeros_like(w_spatial))
    z = jnp.einsum('hts,bnshd->bnthd', ws, vc) + b_spatial.T[None, None, :, :, None]
    return u * z.reshape(bsz, seq, D_SGU)


def _mixer_block(h, w_in, w_pool, b_pool, pool_scale, sgu_ln_g, sgu_ln_b,
                 w_spatial, b_spatial, w_branch_pool, w_branch_sgu, w_out):
    proj = h @ w_in
    a, u, v, ga, gb = jnp.split(proj, SPLITS, axis=-1)
    y_a = _pool_mixer(a, w_pool, b_pool, pool_scale) @ w_branch_pool
    y_b = _sgu_mixer(u, v, sgu_ln_g, sgu_ln_b, w_spatial, b_spatial) @ w_branch_sgu
    merged = jax.nn.sigmoid(ga) * y_a + jax.nn.sigmoid(gb) * y_b
    return merged @ w_out


def _route(xt, w_router, router_bias):
    n_tok = xt.shape[0]
    scores = jax.nn.sigmoid((xt @ w_router).astype(jnp.float32))
    biased = scores + router_bias.astype(jnp.float32)
    grouped = biased.reshape(n_tok, N_GROUPS, N_EXPERTS // N_GROUPS)
    group_score = jnp.sum(lax.top_k(grouped, 2)[0], axis=-1)
    _, top_groups = lax.top_k(group_score, TOPK_GROUPS)
    group_mask = jnp.any(top_groups[:, :, None] == jnp.arange(N_GROUPS)[None, None, :], axis=1)
    expert_mask = jnp.repeat(group_mask, N_EXPERTS // N_GROUPS, axis=1)
    _, top_e = lax.top_k(jnp.where(expert_mask, biased, -jnp.inf), TOP_K)
    wts = jnp.take_along_axis(scores, top_e, axis=1)
    wts = wts / jnp.sum(wts, axis=-1, keepdims=True) * ROUTED_SCALE
    return top_e, wts


def _swiglu(x, w_gate, w_up, w_down):
    return (jax.nn.silu(x @ w_gate) * (x @ w_up)) @ w_down


def _moe(h, w_router, router_bias, w_exp_gate, w_exp_up, w_exp_down,
         w_sh_gate, w_sh_up, w_sh_down):
    bsz, seq, d = h.shape
    n_tok = bsz * seq
    xt = h.reshape(n_tok, d)
    top_e, wts = _route(xt, w_router, router_bias)
    n_pairs = n_tok * TOP_K
    e_flat = top_e.reshape(n_pairs)
    w_flat = wts.reshape(n_pairs)
    tok_flat = jnp.arange(n_pairs, dtype=jnp.int32) // TOP_K
    order = jnp.argsort(e_flat)
    e_sorted = e_flat[order]
    counts = jnp.zeros((N_EXPERTS,), jnp.int32).at[e_flat].add(1)
    starts = jnp.cumsum(counts) - counts
    padded = (counts + MOE_BLOCK - 1) // MOE_BLOCK * MOE_BLOCK
    pends = jnp.cumsum(padded)
    pstarts = pends - padded
    dest = pstarts[e_sorted] + (jnp.arange(n_pairs, dtype=jnp.int32) - starts[e_sorted])
    n_blocks = -(-n_pairs // MOE_BLOCK) + N_EXPERTS
    n_rows = n_blocks * MOE_BLOCK
    row_tok = jnp.full((n_rows,), n_tok, jnp.int32).at[dest].set(tok_flat[order])
    row_w = jnp.zeros((n_rows,), jnp.float32).at[dest].set(w_flat[order])
    block_start = jnp.arange(n_blocks, dtype=jnp.int32) * MOE_BLOCK
    block_e = jnp.minimum(jnp.searchsorted(pends, block_start, side='right'), N_EXPERTS - 1)
    xt_pad = jnp.concatenate([xt, jnp.zeros((1, d), xt.dtype)], axis=0)

    def step(acc, blk):
        toks, bw, e = blk
        xb = xt_pad[toks]
        yb = _swiglu(xb, w_exp_gate[e], w_exp_up[e], w_exp_down[e])
        return acc.at[toks].add(yb.astype(jnp.float32) * bw[:, None]), None

    acc0 = jnp.zeros((n_tok + 1, d), jnp.float32)
    acc, _ = lax.scan(step, acc0, (row_tok.reshape(n_blocks, MOE_BLOCK),
                                   row_w.reshape(n_blocks, MOE_BLOCK), block_e))
    routed = acc[:n_tok].astype(h.dtype)
    shared = _swiglu(xt, w_sh_gate, w_sh_up, w_sh_down)
    return (routed + shared).reshape(bsz, seq, d)


def setup_inputs(seed: int = 0) -> dict:
    key = jax.random.key(seed)
    ks = jax.random.split(key, 24)
    L, D = DEPTH, D_MODEL

    def nrm(k, shape, scale):
        return jax.random.normal(k, shape, jnp.float32) * scale

    return {
        "x": nrm(ks[0], (BATCH, SEQ, D), 1.0),
        "c": nrm(ks[1], (BATCH, D), 1.0),
        "w_ada": nrm(ks[2], (L, D, N_MOD * D), 0.5 * D ** -0.5),
        "b_ada": nrm(ks[3], (L, N_MOD * D), 0.02),
        "w_in": nrm(ks[4], (L, D, D_IN), D ** -0.5),
        "w_pool": nrm(ks[5], (L, N_POOL_GROUPS, POOL_GROUP, POOL_GROUP), POOL_GROUP ** -0.5),
        "b_pool": nrm(ks[6], (L, N_POOL_GROUPS, POOL_GROUP), 0.02),
        "pool_scale": 1.0 + nrm(ks[7], (L, D_POOL), 0.1),
        "sgu_ln_g": 1.0 + nrm(ks[8], (L, D_SGU), 0.1),
        "sgu_ln_b": nrm(ks[9], (L, D_SGU), 0.02),
        "w_spatial": nrm(ks[10], (L, SGU_HEADS, SGU_CHUNK, SGU_CHUNK), SGU_CHUNK ** -0.5),
        "b_spatial": 1.0 + nrm(ks[11], (L, SGU_HEADS, SGU_CHUNK), 0.1),
        "w_branch_pool": nrm(ks[12], (L, D_POOL, D), D_POOL ** -0.5),
        "w_branch_sgu": nrm(ks[13], (L, D_SGU, D), D_SGU ** -0.5),
        "w_out": nrm(ks[14], (L, D, D), D ** -0.5),
        "w_router": nrm(ks[15], (L, D, N_EXPERTS), D ** -0.5),
        "router_bias": nrm(ks[16], (L, N_EXPERTS), 0.01),
        "w_exp_gate": nrm(ks[17], (L, N_EXPERTS, D, D_EXPERT), D ** -0.5),
        "w_exp_up": nrm(ks[18], (L, N_EXPERTS, D, D_EXPERT), D ** -0.5),
        "w_exp_down": nrm(ks[19], (L, N_EXPERTS, D_EXPERT, D), D_EXPERT ** -0.5),
        "w_sh_gate": nrm(ks[20], (L, D, D_SHARED), D ** -0.5),
        "w_sh_up": nrm(ks[21], (L, D, D_SHARED), D ** -0.5),
        "w_sh_down": nrm(ks[22], (L, D_SHARED, D), D_SHARED ** -0.5),
        "final_gain": 1.0 + nrm(ks[23], (D,), 0.1),
    }


def reference(x, c, w_ada, b_ada, w_in, w_pool, b_pool, pool_scale, sgu_ln_g, sgu_ln_b,
              w_spatial, b_spatial, w_branch_pool, w_branch_sgu, w_out, w_router,
              router_bias, w_exp_gate, w_exp_up, w_exp_down, w_sh_gate, w_sh_up,
              w_sh_down, final_gain):
    cond = jax.nn.silu(c)
    for l in range(DEPTH):
        mod = cond @ w_ada[l] + b_ada[l]
        sh1, sc1, g1, sh2, sc2, g2 = jnp.split(mod, N_MOD, axis=-1)
        h = _modulate(_rmsnorm(x), sh1, sc1)
        mix = _mixer_block(h, w_in[l], w_pool[l], b_pool[l], pool_scale[l], sgu_ln_g[l],
                           sgu_ln_b[l], w_spatial[l], b_spatial[l], w_branch_pool[l],
                           w_branch_sgu[l], w_out[l])
        x = x + g1[:, None, :] * mix
        h = _modulate(_rmsnorm(x), sh2, sc2)
        ffn = _moe(h, w_router[l], router_bias[l], w_exp_gate[l], w_exp_up[l],
                   w_exp_down[l], w_sh_gate[l], w_sh_up[l], w_sh_down[l])
        x = x + g2[:, None, :] * ffn
    return _rmsnorm(x) * final_gain
```

```python
import os
import numpy as np
import concourse.bass as bass
import concourse.mybir as mybir
from concourse.bass_utils import run_bass_kernel_spmd

F32 = mybir.dt.float32
BF16 = mybir.dt.bfloat16
AF = mybir.ActivationFunctionType
ALU = mybir.AluOpType
AX = mybir.AxisListType

D = 4096
KC = 32
T = 256
NSUB = 2
SEQ = 4096
NCORES = 8
NE = 65
EPS = 1e-6
BIG = 1.0e4
SLABW = 256
DBG = int(os.environ.get('KDBG', '9'))


class Sem:
    def __init__(self, nc, name):
        self.h = nc.alloc_semaphore(name)
        self.count = 0


class Buf:
    def __init__(self, name):
        self.name = name
        self.last_w = None
        self.readers = []


class Ctx:
    def __init__(self, nc):
        self.nc = nc
        self.eng = {}
        for name, h in (("pe", nc.tensor), ("act", nc.scalar), ("dve", nc.vector),
                        ("pool", nc.gpsimd), ("sp", nc.sync)):
            self.eng[name] = [h, Sem(nc, "e_" + name), {}]
        self.pe_pending = []

    def _wait(self, ename, deps):
        h, _, seen = self.eng[ename]
        best = {}
        for d in deps:
            if d is None:
                continue
            s, v = d
            key = id(s)
            if ename == "pe" and s is self.eng["pe"][1].h:
                continue
            if seen.get(key, 0) >= v:
                continue
            if key not in best or best[key][1] < v:
                best[key] = (s, v)
        for key, (s, v) in best.items():
            h.wait_ge(s, v)
            seen[key] = v

    def _deps(self, r, w):
        deps = []
        for b in r:
            deps.append(b.last_w)
        for b in w:
            assert b not in self.pe_pending, b.name
            deps.append(b.last_w)
            deps.extend(b.readers)
        return deps

    def op(self, ename, fn, r=(), w=(), inc=True):
        self._wait(ename, self._deps(r, w))
        ins = fn()
        h, sem, _ = self.eng[ename]
        if not inc:
            assert ename == "pe"
            for b in r:
                if b not in self.pe_pending:
                    self.pe_pending.append(b)
            return None
        sem.count += 1
        ins.then_inc(sem.h, 1)
        tok = (sem.h, sem.count)
        rr = list(r)
        if ename == "pe":
            rr += self.pe_pending
            self.pe_pending = []
        for b in rr:
            b.readers.append(tok)
        for b in w:
            b.last_w = tok
            b.readers = []
        return tok

    def dma(self, q, out, in_, sem, r=(), w=()):
        self._wait(q, self._deps(r, w))
        h = self.eng[q][0]
        ins = h.dma_start(out=out, in_=in_)
        sem.count += 16
        ins.then_inc(sem.h, 16)
        tok = (sem.h, sem.count)
        for b in r:
            b.readers.append(tok)
        for b in w:
            b.last_w = tok
            b.readers = []
        return tok


ALL_STAGES = ("ada", "s1", "s2", "s3", "s4", "s5", "s6", "s7", "s8", "s9")


def build_nc(ntok, ne=NE, stages=ALL_STAGES):
    nc = bass.Bass("TRN2", target_bir_lowering=False)
    NT = ntok // T
    NE = ne
    shapes = {}
    tiny = set()
    if "ada" not in stages:
        tiny.add("w_ada")
    if not any(x_ in stages for x_ in ("s2", "s3", "s4", "s5")):
        tiny.add("w_in")
    if "s2" not in stages:
        tiny.add("w_pool")
    if "s5" not in stages:
        tiny.update(("w_bp", "w_bs"))
    if "s6" not in stages:
        tiny.add("w_out")
    if "s8" not in stages:
        tiny.update(("w_eg", "w_eu", "w_ed"))

    def din(name, shape, dt=F32):
        shape = list(shape)
        if name in tiny:
            shape[0] = 128
        shapes[name] = tuple(shape)
        return nc.dram_tensor(name, shape, dt, kind="ExternalInput").ap()

    x = din("x", [ntok, D])
    c_in = din("c", [D])
    w_ada = din("w_ada", [D, 6 * D])
    b_ada = din("b_ada", [6 * D])
    w_in = din("w_in", [D, 14336])
    w_pool = din("w_pool", [2048, 512])
    b_pool = din("b_pool", [2048])
    pool_scale = din("pool_scale", [2048])
    ln_g = din("sgu_ln_g", [2048])
    ln_b = din("sgu_ln_b", [2048])
    w_spT = din("w_spT", [16, 128, 128])
    b_sp = din("b_sp", [1, 2048])
    w_bp = din("w_bp", [2048, D])
    w_bs = din("w_bs", [2048, D])
    w_out = din("w_out", [D, D])
    w_router = din("w_router", [D, 64])
    r_bias = din("r_bias", [1, 64])
    w_eg = din("w_eg", [NE * D, 512])
    w_eu = din("w_eu", [NE * D, 512])
    w_ed = din("w_ed", [NE * 512, D])
    fgain_in = din("final_gain", [D])
    ident_in = din("ident", [128, 128])
    tril_in = din("tril", [128, 128])
    corr_in = din("corr0", [128, 64])
    xh_in = din("x_halo", [16, D])
    hs_in = din("hscale", [128, 1])
    out = nc.dram_tensor("out", [ntok, D], F32, kind="ExternalOutput").ap()
    gdT = nc.dram_tensor("gdT", [65, T], F32).ap()

    k = Ctx(nc)

    def sb(name, shape, dt=F32):
        return nc.alloc_sbuf_tensor(name, list(shape), dt)

    ident_f = sb("ident_f", [128, 128]); B_const = Buf("const")
    ones_f = sb("ones_f", [128, 128])
    tril_f = sb("tril_f", [128, 128])
    corr0 = sb("corr0s", [128, 4, 16])
    ones_rb = sb("ones_rb", [1, 128], BF16)
    bsp_b = sb("bsp_b", [1, 2048], BF16)
    c_sb = sb("c_sb", [128, KC])
    condb = sb("condb", [128, KC], BF16)
    mod = sb("mod", [128, 192])
    bada = sb("bada", [128, 192])
    sc1p = sb("sc1p", [128, KC])
    sc2p = sb("sc2p", [128, KC])
    wsT = sb("wsT", [128, 16, 128], BF16)
    lngb = sb("lngb", [128, 2048], BF16)
    lnbb = sb("lnbb", [128, 2048], BF16)
    bpool = sb("bpool", [128, 16])
    pscale = sb("pscale", [128, 16])
    bps = sb("bps", [128, 16])
    rbias = sb("rbias", [128, 64])
    wr_sb = sb("wr_sb", [128, KC, 64])
    fgain = sb("fgain", [128, KC])
    hscale = sb("hscale_s", [128, 1])
    carry = sb("carry", [128, 16, 16]); B_carry = Buf("carry")

    xT = sb("xT", [128, KC, T]); B_xT = Buf("xT")
    hT = sb("hT", [128, KC, T], BF16); B_hT = Buf("hT")
    lntmp = xT[:, 16:24, :].rearrange("p k t -> p (k t)")
    bsp_f = xT[0:1, 24:32, :].rearrange("p k t -> p (k t)")
    yaT = sb("yaT", [128, 16, T], BF16); B_yaT = Buf("yaT")
    sT = sb("sT", [128, 16, T], BF16); B_sT = Buf("sT")
    mrg = sb("mrg", [128, KC, T], BF16); B_mrg = Buf("mrg")
    vn = mrg[:, 0:16, :].rearrange("p k t -> p (k t)").rearrange("p (s f) -> p s f", s=NSUB); B_vn = B_mrg
    aA = sb("aA", [128, 4, 16 + T]); B_aA = Buf("aA")
    aB = sb("aB", [128, 4, 16 + T]); B_aB = Buf("aB")
    aC = sb("aC", [128, 4, 16 + T]); B_aC = Buf("aC")
    diffT = sb("diffT", [128, 4, T], BF16); B_diff = Buf("diff")
    rstd = sb("rstd", [128, T]); B_rstd = Buf("rstd")
    stats = sb("stats", [128, NSUB, 8, 6]); B_stats = Buf("stats")
    mv = sb("mv", [128, NSUB, 2]); B_mv = Buf("mv")
    G_sb = sb("G_sb", [128, NSUB, 65]); B_G = Buf("G")
    GT_sb = sb("GT_sb", [65, T]); B_GT = Buf("GT")
    B_gd = Buf("gd")
    NTMP = 4
    tmps = [(sb(f"tmp{i}", [128, T]), Buf(f"tmp{i}")) for i in range(NTMP)]
    tmp_i = [0]
    rt = [(sb(f"rt{i}", [128, 64]), Buf(f"rt{i}")) for i in range(6)]
    rsm = [(sb(f"rsm{i}", [128, 8]), Buf(f"rsm{i}")) for i in range(6)]
    NGB = 2
    gbts = [(sb(f"gbt{i}", [128, T]), Buf(f"gbt{i}"), Sem(nc, f"gbt{i}")) for i in range(NGB)]
    actT = [(sb(f"actT{i}", [128, 4, T], BF16), Buf(f"actT{i}")) for i in range(2)]
    stg = [(sb(f"stg{i}", [128, 512]), Buf(f"stg{i}"), Sem(nc, f"stg{i}")) for i in range(2)]
    NRING = 2
    ring = [(sb(f"ring{i}", [128, 8192], BF16), Buf(f"ring{i}"), Sem(nc, f"ring{i}")) for i in range(NRING)]
    ring_i = [0]
    psums = [(nc.alloc_psum_tensor(f"ps{i}", [128, 512], F32), Buf(f"ps{i}")) for i in range(8)]
    ps_i = [0]
    s_const = Sem(nc, "const")
    s_gd = Sem(nc, "gd")

    def psum():
        p = psums[ps_i[0] % 8]
        ps_i[0] += 1
        return p

    def tmp():
        t_ = tmps[tmp_i[0] % NTMP]
        tmp_i[0] += 1
        return t_

    V, S, P, G = nc.vector, nc.scalar, nc.tensor, nc.gpsimd
    print('SBUF remaining', nc.sbuf_bytes_remaining)

    with nc.allow_non_contiguous_dma(reason="small const loads"):
        k.dma("sp", ident_f[:, :], ident_in[:, :], s_const, w=[B_const])
        k.dma("sp", tril_f[:, :], tril_in[:, :], s_const, w=[B_const])
        k.dma("sp", corr0[:, :, :], corr_in.rearrange("p (g t) -> p g t", g=4), s_const, w=[B_const])
        k.dma("sp", bsp_f, b_sp[:, :], s_const, w=[B_const, B_xT])
        k.dma("sp", c_sb[:, :], c_in.rearrange("(k p) -> p k", p=128), s_const, w=[B_const])
        k.dma("sp", bada[:, :], b_ada.rearrange("(j p) -> p j", p=128), s_const, w=[B_const])
        k.dma("sp", bpool[:, :], b_pool.rearrange("(j p) -> p j", p=128), s_const, w=[B_const])
        k.dma("sp", pscale[:, :], pool_scale.rearrange("(j p) -> p j", p=128), s_const, w=[B_const])
        k.dma("sp", fgain[:, :], fgain_in.rearrange("(j p) -> p j", p=128), s_const, w=[B_const])
        k.dma("sp", rbias[:, :], r_bias.partition_broadcast(128), s_const, w=[B_const])
        k.dma("sp", hscale[:, :], hs_in[:, :], s_const, w=[B_const])
        k.dma("sp", wr_sb[:, :, :], w_router.rearrange("(k p) e -> p k e", p=128), s_const, w=[B_const])
        k.dma("sp", xT[:, 0:16, 0:128], w_spT.rearrange("h s t -> s h t"), s_const, w=[B_const, B_xT])
        k.dma("sp", lntmp, ln_g.partition_broadcast(128), s_const, w=[B_const, B_xT])
    B_const.last_w = (s_const.h, s_const.count)
    B_xT.last_w = (s_const.h, s_const.count)
    cst = [B_const]
    k.op("dve", lambda: V.memset(ones_f[:, :], 1.0), w=cst)
    k.op("dve", lambda: V.memset(ones_rb[:, :], 1.0), w=cst)
    k.op("dve", lambda: V.memset(carry[:, :, :], 0.0), w=[B_carry])
    k.op("dve", lambda: V.memset(G_sb[:, :, :], 1.0), w=[B_G])
    k.op("dve", lambda: V.tensor_copy(out=bsp_b[:, :], in_=bsp_f), r=cst + [B_xT], w=cst)
    k.op("dve", lambda: V.tensor_copy(out=lngb[:, :], in_=lntmp), r=cst + [B_xT], w=cst)
    with nc.allow_non_contiguous_dma(reason="small const loads"):
        k.dma("sp", lntmp, ln_b.partition_broadcast(128), s_const, r=cst, w=cst + [B_xT])
    k.op("dve", lambda: V.tensor_copy(out=lnbb[:, :], in_=lntmp), r=cst + [B_xT], w=cst)
    for h_ in range(16):
        k.op("dve", lambda h_=h_: V.tensor_tensor(out=wsT[:, h_, :], in0=xT[:, h_, 0:128], in1=tril_f[:, :],
                                                  op=ALU.mult), r=cst + [B_xT], w=cst)
    k.op("dve", lambda: V.tensor_tensor(out=bps[:, :], in0=bpool[:, :], in1=pscale[:, :], op=ALU.mult), r=cst, w=cst)
    k.op("act", lambda: S.activation(out=condb[:, :], in_=c_sb[:, :], func=AF.Silu), r=cst, w=cst)

    def load_slab(w2d, row0, kc, col0, ncols):
        sl = ring[ring_i[0] % NRING]
        ring_i[0] += 1
        t_, b_, s_ = sl
        view = t_[:, 0:kc * ncols].rearrange("p (k n) -> p k n", k=kc)
        src = w2d[row0:row0 + kc * 128, col0:col0 + ncols].rearrange("(k p) n -> p k n", p=128)
        k.dma("pool", view, src, s_, w=[b_])
        return view, b_

    def stream(slabs, body):
        loaded = []
        nxt = 0
        depth = NRING - 1
        while nxt < min(depth, len(slabs)):
            loaded.append(load_slab(*slabs[nxt][:5])); nxt += 1
        for i, sl in enumerate(slabs):
            view, b_ = loaded[i]
            body(view, b_, sl[5])
            if nxt < len(slabs):
                loaded.append(load_slab(*slabs[nxt][:5])); nxt += 1

    def mm_fm(w2d, row0, kc, col0, ncols_total, rhs_fn, rhs_bufs, evac, nfree=T):
        slabs = [(w2d, row0, kc, col0 + j * SLABW, SLABW, j) for j in range(ncols_total // SLABW)]

        def body(view, b_, j):
            for ml in range(SLABW // 128):
                pt, pb = psum()
                for kk in range(kc):
                    k.op("pe", lambda kk=kk: P.matmul(pt[:, 0:nfree], lhsT=view[:, kk, ml * 128:(ml + 1) * 128],
                                                      rhs=rhs_fn(kk), start=(kk == 0), stop=(kk == kc - 1)),
                         r=[b_] + rhs_bufs, w=[pb], inc=(kk == kc - 1))
                evac(j * (SLABW // 128) + ml, pt, pb)
        stream(slabs, body)

    if "ada" in stages:
        modps, modpb = psum()

        def ada_body(view, b_, j):
            for ml in range(2):
                col = j * 2 + ml
                for kk in range(KC):
                    k.op("pe", lambda kk=kk: P.matmul(modps[:, col:col + 1], lhsT=view[:, kk, ml * 128:(ml + 1) * 128],
                                                      rhs=condb[:, kk:kk + 1], start=(kk == 0), stop=(kk == KC - 1)),
                         r=[b_] + cst, w=[modpb], inc=(kk == KC - 1))
        stream([(w_ada, 0, KC, j * SLABW, SLABW, j) for j in range(6 * D // SLABW)], ada_body)
        k.op("dve", lambda: V.tensor_tensor(out=mod[:, :], in0=modps[:, 0:192], in1=bada[:, :], op=ALU.add),
             r=[modpb] + cst, w=cst)
        k.op("dve", lambda: V.tensor_scalar(out=sc1p[:, :], in0=mod[:, 32:64], scalar1=1.0, scalar2=None, op0=ALU.add),
             r=cst, w=cst)
        k.op("dve", lambda: V.tensor_scalar(out=sc2p[:, :], in0=mod[:, 128:160], scalar1=1.0, scalar2=None, op0=ALU.add),
             r=cst, w=cst)
    sh1 = mod[:, 0:32]; g1 = mod[:, 64:96]; sh2 = mod[:, 96:128]; g2 = mod[:, 160:192]

    def rms_stats():
        pt, pb = psum()
        for kk in range(KC):
            tt, tb = tmp()
            k.op("act", lambda: S.activation(out=tt[:, :], in_=xT[:, kk, :], func=AF.Square), r=[B_xT], w=[tb])
            k.op("pe", lambda: P.matmul(pt[:, 0:T], lhsT=ones_f[:, :], rhs=tt[:, :], start=(kk == 0),
                                        stop=(kk == KC - 1)), r=[tb] + cst, w=[pb], inc=True)
        k.op("dve", lambda: V.tensor_scalar(out=rstd[:, :], in0=pt[:, 0:T], scalar1=1.0 / D, scalar2=EPS,
                                            op0=ALU.mult, op1=ALU.add), r=[pb], w=[B_rstd])
        k.op("act", lambda: S.activation(out=rstd[:, :], in_=rstd[:, :], func=AF.Sqrt), r=[B_rstd], w=[B_rstd])
        k.op("dve", lambda: V.reciprocal(out=rstd[:, :], in_=rstd[:, :]), r=[B_rstd], w=[B_rstd])

    if "s2" in stages:
        for cpc in range(8):
            st, stb, sts = stg[cpc % 2]
            k.dma("sp", st[0:16, :], xh_in[:, cpc * 512:(cpc + 1) * 512], sts, w=[stb])
            pt, pb = psum()
            for j in range(4):
                k.op("pe", lambda j=j: P.transpose(out=pt[:, j * 16:(j + 1) * 16], in_=st[0:16, j * 128:(j + 1) * 128],
                                                   identity=ident_f[0:16, 0:16]), r=[stb] + cst, w=[pb], inc=(j == 3))
            k0 = cpc * 4
            k.op("act", lambda: S.copy(out=xT[:, k0:k0 + 4, 0:16], in_=pt[:, 0:64].rearrange("p (a b) -> p a b", a=4)),
                 r=[pb], w=[B_xT])
        rms_stats()
        for kk in range(KC):
            tt, tb = tmp()
            k.op("dve", lambda: V.tensor_tensor(out=tt[:, :], in0=xT[:, kk, :], in1=rstd[:, :], op=ALU.mult),
                 r=[B_xT, B_rstd], w=[tb])
            k.op("act", lambda: S.activation(out=hT[:, kk, :], in_=tt[:, :], func=AF.Identity,
                                             bias=sh1[:, kk:kk + 1], scale=sc1p[:, kk:kk + 1]),
                 r=[tb] + cst, w=[B_hT])

        def evac_halo(m, pt, pb):
            k.op("dve", lambda: V.tensor_scalar(out=carry[:, m, :], in0=pt[:, 0:16], scalar1=hscale[:, 0:1], scalar2=None,
                                                op0=ALU.mult), r=[pb] + cst, w=[B_carry])
        mm_fm(w_in, 0, KC, 0, 2048, lambda kk: hT[:, kk, 0:16], [B_hT], evac_halo, nfree=16)

    for it in range(NT):
        t0 = it * T
        if "s1" in stages:
            for blk in range(NSUB):
                for cpc in range(8):
                    st, stb, sts = stg[(blk * 8 + cpc) % 2]
                    k.dma("sp", st[:, :], x[t0 + blk * 128:t0 + (blk + 1) * 128, cpc * 512:(cpc + 1) * 512], sts, w=[stb])
                    pt, pb = psum()
                    for j in range(4):
                        k.op("pe", lambda j=j: P.transpose(out=pt[:, j * 128:(j + 1) * 128],
                                                           in_=st[:, j * 128:(j + 1) * 128],
                                                           identity=ident_f[:, :]),
                             r=[stb] + cst, w=[pb], inc=(j == 3))
                    k0 = cpc * 4
                    k.op("act", lambda: S.copy(out=xT[:, k0:k0 + 4, blk * 128:(blk + 1) * 128],
                                               in_=pt[:, 0:512].rearrange("p (a b) -> p a b", a=4)),
                         r=[pb], w=[B_xT])
            rms_stats()
            for kk in range(KC):
                tt, tb = tmp()
                k.op("dve", lambda: V.tensor_tensor(out=tt[:, :], in0=xT[:, kk, :], in1=rstd[:, :], op=ALU.mult),
                     r=[B_xT, B_rstd], w=[tb])
                k.op("act", lambda: S.activation(out=hT[:, kk, :], in_=tt[:, :], func=AF.Identity,
                                                 bias=sh1[:, kk:kk + 1], scale=sc1p[:, kk:kk + 1]),
                     r=[tb] + cst, w=[B_hT])

        if "s2" in stages:
            for g in range(4):
                def evac_a(m, pt, pb):
                    k.op("act", lambda: S.copy(out=aA[:, m, 16:16 + T], in_=pt[:, 0:T]), r=[pb], w=[B_aA])
                mm_fm(w_in, 0, KC, g * 512, 512, lambda kk: hT[:, kk, :], [B_hT], evac_a)
                k.op("dve", lambda: V.tensor_copy(out=aA[:, :, 0:16], in_=carry[:, g * 4:(g + 1) * 4, :]),
                     r=[B_carry], w=[B_aA])
                L = 16 + T
                k.op("dve", lambda: V.tensor_tensor(out=aB[:, :, 1:L], in0=aA[:, :, 1:L], in1=aA[:, :, 0:L - 1], op=ALU.add),
                     r=[B_aA], w=[B_aB])
                src_t, src_b = aB, B_aB
                if g >= 1:
                    k.op("dve", lambda: V.tensor_tensor(out=aC[:, :, 3:L], in0=aB[:, :, 3:L], in1=aB[:, :, 1:L - 2], op=ALU.add),
                         r=[B_aB], w=[B_aC])
                    src_t, src_b = aC, B_aC
                if g >= 2:
                    k.op("dve", lambda: V.tensor_tensor(out=aB[:, :, 7:L], in0=aC[:, :, 7:L], in1=aC[:, :, 3:L - 4], op=ALU.add),
                         r=[B_aC], w=[B_aB])
                    src_t, src_b = aB, B_aB
                if g >= 3:
                    k.op("dve", lambda: V.tensor_tensor(out=aC[:, :, 15:L], in0=aB[:, :, 15:L], in1=aB[:, :, 7:L - 8], op=ALU.add),
                         r=[B_aB], w=[B_aC])
                    src_t, src_b = aC, B_aC
                wdw = float(2 ** (g + 1))
                if it == 0:
                    k.op("dve", lambda: V.tensor_tensor(out=src_t[:, :, 16:32], in0=src_t[:, :, 16:32],
                                                        in1=corr0[:, g:g + 1, :].to_broadcast([128, 4, 16]), op=ALU.mult),
                         r=[src_b] + cst, w=[src_b])
                k.op("dve", lambda: V.scalar_tensor_tensor(out=diffT[:, :, :], in0=src_t[:, :, 16:L], scalar=1.0 / wdw,
                                                           in1=aA[:, :, 16:L], op0=ALU.mult, op1=ALU.subtract),
                     r=[src_b, B_aA], w=[B_diff])
                k.op("dve", lambda: V.tensor_copy(out=carry[:, g * 4:(g + 1) * 4, :], in_=aA[:, :, T:T + 16]),
                     r=[B_aA], w=[B_carry])

                def evac_ya(m, pt, pb):
                    mm = g * 4 + m
                    k.op("dve", lambda: V.tensor_scalar(out=yaT[:, mm, :], in0=pt[:, 0:T], scalar1=pscale[:, mm:mm + 1],
                                                        scalar2=bps[:, mm:mm + 1], op0=ALU.mult, op1=ALU.add),
                         r=[pb] + cst, w=[B_yaT])
                mm_fm(w_pool, g * 512, 4, 0, 512, lambda kk: diffT[:, kk, :], [B_diff], evac_ya)

        if "s3" in stages:
            def v_body(view, b_, j):
                for blk in range(NSUB):
                    pt, pb = psum()
                    for kk in range(KC):
                        k.op("pe", lambda kk=kk: P.matmul(pt[:, 0:SLABW], lhsT=hT[:, kk, blk * 128:(blk + 1) * 128],
                                                          rhs=view[:, kk, :], start=(kk == 0), stop=(kk == KC - 1)),
                             r=[b_, B_hT], w=[pb], inc=(kk == KC - 1))
                    if DBG < 2:
                        continue
                    qt, qb = tmp()
                    k.op("act", lambda: S.activation(out=qt[:, :], in_=pt[:, 0:SLABW], func=AF.Square), r=[pb], w=[qb])
                    k.op("dve", lambda: V.tensor_reduce(out=stats[:, blk, j, 0:1], in_=pt[:, 0:SLABW], axis=AX.X, op=ALU.add),
                         r=[pb, qb], w=[B_stats])
                    k.op("dve", lambda: V.tensor_reduce(out=stats[:, blk, j, 1:2], in_=qt[:, :], axis=AX.X, op=ALU.add),
                         r=[qb], w=[B_stats])
                    k.op("act", lambda: S.copy(out=vn[:, blk, j * SLABW:(j + 1) * SLABW], in_=pt[:, 0:SLABW]),
                         r=[pb], w=[B_vn])
            stream([(w_in, 0, KC, 4096 + j * SLABW, SLABW, j) for j in range(8)], v_body)
            if DBG >= 3:
                k.op("dve", lambda: V.tensor_reduce(out=mv[:, :, 0], in_=stats[:, :, :, 0], axis=AX.X, op=ALU.add),
                     r=[B_stats], w=[B_mv])
                k.op("dve", lambda: V.tensor_reduce(out=mv[:, :, 1], in_=stats[:, :, :, 1], axis=AX.X, op=ALU.add),
                     r=[B_stats], w=[B_mv])
                k.op("dve", lambda: V.tensor_scalar(out=mv[:, :, :], in0=mv[:, :, :], scalar1=1.0 / 2048.0, scalar2=None,
                                                    op0=ALU.mult), r=[B_mv], w=[B_mv])
                k.op("dve", lambda: V.tensor_tensor(out=stats[:, :, 0, 2], in0=mv[:, :, 0], in1=mv[:, :, 0], op=ALU.mult),
                     r=[B_mv], w=[B_stats])
                k.op("dve", lambda: V.tensor_tensor(out=mv[:, :, 1], in0=mv[:, :, 1], in1=stats[:, :, 0, 2], op=ALU.subtract),
                     r=[B_mv, B_stats], w=[B_mv])
                k.op("dve", lambda: V.tensor_scalar(out=mv[:, :, 1], in0=mv[:, :, 1], scalar1=EPS, scalar2=None,
                                                    op0=ALU.add), r=[B_mv], w=[B_mv])
                k.op("act", lambda: S.activation(out=mv[:, :, 1], in_=mv[:, :, 1], func=AF.Sqrt), r=[B_mv], w=[B_mv])
                k.op("dve", lambda: V.reciprocal(out=mv[:, :, 1], in_=mv[:, :, 1]), r=[B_mv], w=[B_mv])
            for blk in range(NSUB):
                if DBG < 4:
                    continue
                k.op("dve", lambda: V.tensor_scalar(out=vn[:, blk, :], in0=vn[:, blk, :], scalar1=mv[:, blk, 0:1],
                                                    scalar2=mv[:, blk, 1:2], op0=ALU.subtract, op1=ALU.mult),
                     r=[B_mv, B_vn], w=[B_vn])
                k.op("dve", lambda: V.tensor_tensor(out=vn[:, blk, :], in0=vn[:, blk, :], in1=lngb[:, :], op=ALU.mult),
                     r=[B_vn] + cst, w=[B_vn])
                k.op("dve", lambda: V.tensor_tensor(out=vn[:, blk, :], in0=vn[:, blk, :], in1=lnbb[:, :], op=ALU.add),
                     r=[B_vn] + cst, w=[B_vn])

        if "s4" in stages:
            def evac_u(hh, pt, pb):
                ut, ub = tmp()
                k.op("act", lambda: S.copy(out=ut[:, :], in_=pt[:, 0:T]), r=[pb], w=[ub])
                zt, zb = psum()
                for blk in range(NSUB):
                    k.op("pe", lambda: P.matmul(zt[:, blk * 128:(blk + 1) * 128], lhsT=vn[:, blk, hh * 128:(hh + 1) * 128],
                                                rhs=wsT[:, hh, :], start=True, stop=False), r=[B_vn] + cst, w=[zb], inc=False)
                    k.op("pe", lambda: P.matmul(zt[:, blk * 128:(blk + 1) * 128], lhsT=ones_rb[0:1, :],
                                                rhs=bsp_b[0:1, hh * 128:(hh + 1) * 128], start=False, stop=True),
                         r=cst, w=[zb], inc=(blk == NSUB - 1))
                k.op("dve", lambda: V.tensor_tensor(out=sT[:, hh, :], in0=zt[:, 0:T], in1=ut[:, :], op=ALU.mult),
                     r=[zb, ub], w=[B_sT])
            mm_fm(w_in, 0, KC, 2048, 2048, lambda kk: hT[:, kk, :], [B_hT], evac_u)

        if "s5" in stages:
            for mp in range(16):
                c0 = mp * SLABW
                slabs = [(w_in, 0, KC, 6144 + c0, SLABW, 0), (w_bp, 0, 16, c0, SLABW, 1),
                         (w_in, 0, KC, 10240 + c0, SLABW, 2), (w_bs, 0, 16, c0, SLABW, 3)]
                res = {}

                def s5_body(view, b_, kind):
                    kc_ = KC if kind in (0, 2) else 16
                    src = {0: (hT, B_hT), 1: (yaT, B_yaT), 2: (hT, B_hT), 3: (sT, B_sT)}[kind]
                    for ml in range(2):
                        pt, pb = psum()
                        for kk in range(kc_):
                            k.op("pe", lambda kk=kk: P.matmul(pt[:, 0:T], lhsT=view[:, kk, ml * 128:(ml + 1) * 128],
                                                              rhs=src[0][:, kk, :], start=(kk == 0), stop=(kk == kc_ - 1)),
                                 r=[b_, src[1]], w=[pb], inc=(kk == kc_ - 1))
                        if kind in (0, 2):
                            st_, sb_ = tmp()
                            k.op("act", lambda: S.activation(out=st_[:, :], in_=pt[:, 0:T], func=AF.Sigmoid), r=[pb], w=[sb_])
                            res[(kind, ml)] = (st_, sb_)
                        else:
                            gt_, gb_ = res[(kind - 1, ml)]
                            m = mp * 2 + ml
                            if kind == 1:
                                k.op("dve", lambda: V.tensor_tensor(out=gt_[:, :], in0=gt_[:, :], in1=pt[:, 0:T], op=ALU.mult),
                                     r=[pb, gb_], w=[gb_])
                                res[("a", ml)] = (gt_, gb_)
                            else:
                                at_, ab_ = res[("a", ml)]
                                k.op("dve", lambda: V.tensor_tensor(out=gt_[:, :], in0=gt_[:, :], in1=pt[:, 0:T], op=ALU.mult),
                                     r=[pb, gb_], w=[gb_])
                                k.op("dve", lambda: V.tensor_tensor(out=mrg[:, m, :], in0=gt_[:, :], in1=at_[:, :], op=ALU.add),
                                     r=[gb_, ab_], w=[B_mrg])
                stream(slabs, s5_body)

        if "s6" in stages:
            def evac_x1(m, pt, pb):
                k.op("dve", lambda: V.scalar_tensor_tensor(out=xT[:, m, :], in0=pt[:, 0:T], scalar=g1[:, m:m + 1],
                                                           in1=xT[:, m, :], op0=ALU.mult, op1=ALU.add),
                     r=[pb, B_xT] + cst, w=[B_xT])
            mm_fm(w_out, 0, KC, 0, D, lambda kk: mrg[:, kk, :], [B_mrg], evac_x1)

        if "s7" in stages:
            rms_stats()
            lgs = [psum() for _ in range(NSUB)]
            for kk in range(KC):
                tt, tb = tmp()
                k.op("dve", lambda: V.tensor_tensor(out=tt[:, :], in0=xT[:, kk, :], in1=rstd[:, :], op=ALU.mult),
                     r=[B_xT, B_rstd], w=[tb])
                k.op("act", lambda: S.activation(out=tt[:, :], in_=tt[:, :], func=AF.Identity,
                                                 bias=sh2[:, kk:kk + 1], scale=sc2p[:, kk:kk + 1]), r=[tb] + cst, w=[tb])
                k.op("dve", lambda: V.tensor_copy(out=hT[:, kk, :], in_=tt[:, :]), r=[tb], w=[B_hT])
                for blk in range(NSUB):
                    k.op("pe", lambda: P.matmul(lgs[blk][0][:, 0:64], lhsT=tt[:, blk * 128:(blk + 1) * 128],
                                                rhs=wr_sb[:, kk, :], start=(kk == 0), stop=(kk == KC - 1)),
                         r=[tb] + cst, w=[lgs[blk][1]], inc=True)
            for blk in range(NSUB):
                (sc_, scb), (bi_, bib), (t3_, t3b), (ms_, msb), (sel_, selb), (w_, wb) = rt
                (m1_, m1b), (m2_, m2b), (gs_, gsb), (g8_, g8b), (gm_, gmb), (e8_, e8b) = rsm
                k.op("act", lambda: S.activation(out=sc_[:, :], in_=lgs[blk][0][:, 0:64], func=AF.Sigmoid),
                     r=[lgs[blk][1]], w=[scb])
                k.op("dve", lambda: V.tensor_tensor(out=bi_[:, :], in0=sc_[:, :], in1=rbias[:, :], op=ALU.add),
                     r=[scb] + cst, w=[bib])
                bi3 = bi_[:, :].rearrange("p (g e) -> p g e", g=8)
                t33 = t3_[:, :].rearrange("p (g e) -> p g e", g=8)
                k.op("dve", lambda: V.tensor_reduce(out=m1_[:, :], in_=bi3, axis=AX.X, op=ALU.max), r=[bib], w=[m1b])
                k.op("dve", lambda: V.tensor_tensor(out=t33, in0=bi3, in1=m1_[:, :].unsqueeze(2).to_broadcast([128, 8, 8]),
                                                    op=ALU.is_equal), r=[bib, m1b], w=[t3b])
                k.op("dve", lambda: V.scalar_tensor_tensor(out=t3_[:, :], in0=t3_[:, :], scalar=-BIG, in1=bi_[:, :],
                                                           op0=ALU.mult, op1=ALU.add), r=[t3b, bib], w=[t3b])
                k.op("dve", lambda: V.tensor_reduce(out=m2_[:, :], in_=t33, axis=AX.X, op=ALU.max), r=[t3b], w=[m2b])
                k.op("dve", lambda: V.tensor_tensor(out=gs_[:, :], in0=m1_[:, :], in1=m2_[:, :], op=ALU.add),
                     r=[m1b, m2b], w=[gsb])
                k.op("dve", lambda: V.max(out=g8_[:, :], in_=gs_[:, :]), r=[gsb], w=[g8b])
                k.op("dve", lambda: V.tensor_scalar(out=gm_[:, :], in0=gs_[:, :], scalar1=g8_[:, 3:4], scalar2=None,
                                                    op0=ALU.is_ge), r=[gsb, g8b], w=[gmb])
                k.op("dve", lambda: V.tensor_scalar(out=gm_[:, :], in0=gm_[:, :], scalar1=-1.0, scalar2=BIG,
                                                    op0=ALU.add, op1=ALU.mult), r=[gmb], w=[gmb])
                ms3 = ms_[:, :].rearrange("p (g e) -> p g e", g=8)
                k.op("dve", lambda: V.tensor_tensor(out=ms3, in0=bi3, in1=gm_[:, :].unsqueeze(2).to_broadcast([128, 8, 8]),
                                                    op=ALU.add), r=[bib, gmb], w=[msb])
                k.op("dve", lambda: V.max(out=e8_[:, :], in_=ms_[:, :]), r=[msb], w=[e8b])
                k.op("dve", lambda: V.tensor_scalar(out=sel_[:, :], in0=ms_[:, :], scalar1=e8_[:, 7:8], scalar2=None,
                                                    op0=ALU.is_ge), r=[msb, e8b], w=[selb])
                k.op("dve", lambda: V.tensor_tensor(out=w_[:, :], in0=sc_[:, :], in1=sel_[:, :], op=ALU.mult),
                     r=[scb, selb], w=[wb])
                k.op("dve", lambda: V.tensor_reduce(out=m1_[:, 0:1], in_=w_[:, :], axis=AX.X, op=ALU.add), r=[wb], w=[m1b])
                k.op("dve", lambda: V.reciprocal(out=m1_[:, 0:1], in_=m1_[:, 0:1]), r=[m1b], w=[m1b])
                k.op("dve", lambda: V.tensor_scalar(out=G_sb[:, blk, 0:64], in0=w_[:, :], scalar1=m1_[:, 0:1], scalar2=2.5,
                                                    op0=ALU.mult, op1=ALU.mult), r=[wb, m1b], w=[B_G])
                pt, pb = psum()
                k.op("pe", lambda: P.transpose(out=pt[0:65, 0:128], in_=G_sb[:, blk, :], identity=ident_f[:, :]),
                     r=[B_G] + cst, w=[pb])
                k.op("act", lambda: S.copy(out=GT_sb[:, blk * 128:(blk + 1) * 128], in_=pt[0:65, 0:128]), r=[pb], w=[B_GT])
            k.dma("sp", gdT[:, :], GT_sb[:, :], s_gd, r=[B_GT], w=[B_gd])

        if "s8" in stages:
            slabs = []
            for e in range(NE):
                for fp in range(2):
                    slabs.append((w_eg, e * D, KC, fp * SLABW, SLABW, (e, "g", fp)))
                    slabs.append((w_eu, e * D, KC, fp * SLABW, SLABW, (e, "u", fp)))
                for dh in range(2):
                    slabs.append((w_ed, e * 512, 4, dh * 2048, 2048, (e, "d", dh)))
            st8 = {}

            def moe_body(view, b_, info):
                e, kind, idx = info
                at_, ab_ = actT[e % 2]
                if kind == "g":
                    st8["g"] = []
                    for fl in range(2):
                        pt, pb = psum()
                        for kk in range(KC):
                            k.op("pe", lambda kk=kk: P.matmul(pt[:, 0:T], lhsT=view[:, kk, fl * 128:(fl + 1) * 128],
                                                              rhs=hT[:, kk, :], start=(kk == 0), stop=(kk == KC - 1)),
                                 r=[b_, B_hT], w=[pb], inc=(kk == KC - 1))
                        gt_, gb_ = tmp()
                        k.op("act", lambda: S.activation(out=gt_[:, :], in_=pt[:, 0:T], func=AF.Silu), r=[pb], w=[gb_])
                        st8["g"].append((gt_, gb_))
                    if idx == 0:
                        gbt, gbb, gbs = gbts[e % NGB]
                        with nc.allow_non_contiguous_dma(reason="bcast"):
                            k.dma("sp", gbt[:, :], gdT[(e if e < NE - 1 else 64):(e if e < NE - 1 else 64) + 1, :].partition_broadcast(128), gbs, r=[B_gd], w=[gbb])
                elif kind == "u":
                    gbt, gbb, gbs = gbts[e % NGB]
                    for fl in range(2):
                        pt, pb = psum()
                        for kk in range(KC):
                            k.op("pe", lambda kk=kk: P.matmul(pt[:, 0:T], lhsT=view[:, kk, fl * 128:(fl + 1) * 128],
                                                              rhs=hT[:, kk, :], start=(kk == 0), stop=(kk == KC - 1)),
                                 r=[b_, B_hT], w=[pb], inc=(kk == KC - 1))
                        gt_, gb_ = st8["g"][fl]
                        k.op("dve", lambda: V.tensor_tensor(out=gt_[:, :], in0=gt_[:, :], in1=pt[:, 0:T], op=ALU.mult),
                             r=[pb, gb_], w=[gb_])
                        k.op("dve", lambda: V.tensor_tensor(out=at_[:, idx * 2 + fl, :], in0=gt_[:, :], in1=gbt[:, :],
                                                            op=ALU.mult), r=[gb_, gbb], w=[ab_])
                else:
                    for ml in range(16):
                        m = idx * 16 + ml
                        pt, pb = psum()
                        for kk in range(4):
                            k.op("pe", lambda kk=kk: P.matmul(pt[:, 0:T], lhsT=view[:, kk, ml * 128:(ml + 1) * 128],
                                                              rhs=at_[:, kk, :], start=(kk == 0), stop=(kk == 3)),
                                 r=[b_, ab_], w=[pb], inc=(kk == 3))
                        k.op("dve", lambda: V.scalar_tensor_tensor(out=xT[:, m, :], in0=pt[:, 0:T], scalar=g2[:, m:m + 1],
                                                                   in1=xT[:, m, :], op0=ALU.mult, op1=ALU.add),
                             r=[pb, B_xT] + cst, w=[B_xT])
            stream(slabs, moe_body)

        if "s9" in stages:
            rms_stats()
            for blk in range(NSUB):
                for cpc in range(8):
                    st, stb, sts = stg[(blk * 8 + cpc) % 2]
                    pt, pb = psum()
                    for j in range(4):
                        kk = cpc * 4 + j
                        ot, ob = tmp()
                        k.op("dve", lambda: V.scalar_tensor_tensor(out=ot[:, 0:128], in0=xT[:, kk, blk * 128:(blk + 1) * 128],
                                                                   scalar=fgain[:, kk:kk + 1],
                                                                   in1=rstd[:, blk * 128:(blk + 1) * 128],
                                                                   op0=ALU.mult, op1=ALU.mult),
                             r=[B_xT, B_rstd] + cst, w=[ob])
                        k.op("pe", lambda: P.transpose(out=pt[:, j * 128:(j + 1) * 128], in_=ot[:, 0:128],
                                                       identity=ident_f[:, :]), r=[ob] + cst, w=[pb], inc=True)
                    k.op("act", lambda: S.copy(out=st[:, :], in_=pt[:, 0:512]), r=[pb], w=[stb])
                    k.dma("sp", out[t0 + blk * 128:t0 + (blk + 1) * 128, cpc * 512:(cpc + 1) * 512], st[:, :], sts,
                          r=[stb], w=[stb])

    for st, stb, sts in stg:
        nc.sync.wait_ge(sts.h, sts.count)
    nc._in_shapes = shapes
    return nc


_CACHE = {}


def _host_consts():
    ident = np.eye(128, dtype=np.float32)
    s = np.arange(128)[:, None]; t = np.arange(128)[None, :]
    tril = (s <= t).astype(np.float32)
    corr = np.zeros((4, 16), np.float32)
    for g, w in enumerate((2, 4, 8, 16)):
        for p in range(16):
            corr[g, p] = w / min(p + 1, w)
    corr0 = np.ascontiguousarray(np.broadcast_to(corr.reshape(1, 64), (128, 64))).astype(np.float32)
    return ident, tril, corr0


def kernel(x, c, w_ada, b_ada, w_in, w_pool, b_pool, pool_scale, sgu_ln_g, sgu_ln_b, w_spatial, b_spatial,
           w_branch_pool, w_branch_sgu, w_out, w_router, router_bias, w_exp_gate, w_exp_up, w_exp_down,
           w_sh_gate, w_sh_up, w_sh_down, final_gain, _ntok=SEQ, _ncores=NCORES, _ne=NE, _stages=ALL_STAGES, _start=0):
    f = lambda a: np.ascontiguousarray(np.asarray(a, dtype=np.float32))
    ident, tril, corr0 = _host_consts()
    shared = dict(
        w_ada=f(w_ada[0]), b_ada=f(b_ada[0]), w_in=f(w_in[0]), w_pool=f(w_pool[0]).reshape(2048, 512),
        b_pool=f(b_pool[0]).reshape(2048), pool_scale=f(pool_scale[0]), sgu_ln_g=f(sgu_ln_g[0]), sgu_ln_b=f(sgu_ln_b[0]),
        w_spT=f(np.transpose(np.asarray(w_spatial[0]), (0, 2, 1))), b_sp=f(b_spatial[0]).reshape(1, 2048),
        w_bp=f(w_branch_pool[0]), w_bs=f(w_branch_sgu[0]), w_out=f(w_out[0]), w_router=f(w_router[0]),
        r_bias=f(router_bias[0]).reshape(1, 64),
        w_eg=f(np.concatenate([np.asarray(w_exp_gate[0]).reshape(64 * D, 512)[:(_ne - 1) * D], np.asarray(w_sh_gate[0])], 0)),
        w_eu=f(np.concatenate([np.asarray(w_exp_up[0]).reshape(64 * D, 512)[:(_ne - 1) * D], np.asarray(w_sh_up[0])], 0)),
        w_ed=f(np.concatenate([np.asarray(w_exp_down[0]).reshape(64 * 512, D)[:(_ne - 1) * 512], np.asarray(w_sh_down[0])], 0)),
        final_gain=f(final_gain), ident=ident, tril=tril, corr0=corr0,
    )
    xs = np.asarray(x, dtype=np.float32)
    cs = np.asarray(c, dtype=np.float32)
    nsplit = max(1, _ncores // 4)
    per = min(_ntok, SEQ // nsplit)
    ones_corr = np.ones((128, 64), np.float32)
    in_maps = []
    for cidx in range(_ncores):
        b, half = cidx // nsplit, cidx % nsplit
        m = dict(shared)
        lo = half * per + _start
        m["x"] = np.ascontiguousarray(xs[b, lo:lo + per])
        m["c"] = np.ascontiguousarray(cs[b])
        if lo == 0:
            m["x_halo"] = np.zeros((16, D), np.float32)
            m["hscale"] = np.zeros((128, 1), np.float32)
        else:
            m["x_halo"] = np.ascontiguousarray(xs[b, lo - 16:lo])
            m["hscale"] = np.ones((128, 1), np.float32)
            m["corr0"] = ones_corr
        in_maps.append(m)
    _ntok = per
    key = (_ntok, _ne, tuple(_stages))
    if key not in _CACHE:
        _CACHE[key] = build_nc(_ntok, _ne, tuple(_stages))
    nc = _CACHE[key]
    for m in in_maps:
        for name, shp in nc._in_shapes.items():
            if m[name].shape != shp:
                m[name] = np.ascontiguousarray(m[name][:shp[0]])
    res = run_bass_kernel_spmd(nc, in_maps, core_ids=list(range(_ncores)))
    outs = [np.asarray(r["out"], dtype=np.float32) for r in res.results]
    nb = _ncores // nsplit
    return np.stack([np.concatenate(outs[b * nsplit:(b + 1) * nsplit], 0) for b in range(nb)], 0)
```

```python
import os
import numpy as np
import concourse.bass as bass
import concourse.mybir as mybir
from concourse.bass_utils import run_bass_kernel_spmd

F32 = mybir.dt.float32
BF16 = mybir.dt.bfloat16
AF = mybir.ActivationFunctionType
ALU = mybir.AluOpType
AX = mybir.AxisListType

D = 4096
KC = 32
T = 256
NSUB = 2
SEQ = 4096
NCORES = 8
NE = 65
EPS = 1e-6
BIG = 1.0e4
SLABW = 256
DBG = int(os.environ.get('KDBG', '9'))


class Sem:
    def __init__(self, nc, name):
        self.h = nc.alloc_semaphore(name)
        self.count = 0


class Buf:
    def __init__(self, name):
        self.name = name
        self.last_w = None
        self.readers = []


class Ctx:
    def __init__(self, nc):
        self.nc = nc
        self.eng = {}
        for name, h in (("pe", nc.tensor), ("act", nc.scalar), ("dve", nc.vector),
                        ("pool", nc.gpsimd), ("sp", nc.sync)):
            self.eng[name] = [h, Sem(nc, "e_" + name), {}]
        self.pe_pending = []

    def _wait(self, ename, deps):
        h, _, seen = self.eng[ename]
        best = {}
        for d in deps:
            if d is None:
                continue
            s, v = d
            key = id(s)
            if ename == "pe" and s is self.eng["pe"][1].h:
                continue
            if seen.get(key, 0) >= v:
                continue
            if key not in best or best[key][1] < v:
                best[key] = (s, v)
        for key, (s, v) in best.items():
            h.wait_ge(s, v)
            seen[key] = v

    def _deps(self, r, w):
        deps = []
        for b in r:
            deps.append(b.last_w)
        for b in w:
            assert b not in self.pe_pending, b.name
            deps.append(b.last_w)
            deps.extend(b.readers)
        return deps

    def op(self, ename, fn, r=(), w=(), inc=True):
        self._wait(ename, self._deps(r, w))
        ins = fn()
        h, sem, _ = self.eng[ename]
        if not inc:
            assert ename == "pe"
            for b in r:
                if b not in self.pe_pending:
                    self.pe_pending.append(b)
            return None
        sem.count += 1
        ins.then_inc(sem.h, 1)
        tok = (sem.h, sem.count)
        rr = list(r)
        if ename == "pe":
            rr += self.pe_pending
            self.pe_pending = []
        for b in rr:
            b.readers.append(tok)
        for b in w:
            b.last_w = tok
            b.readers = []
        return tok

    def dma(self, q, out, in_, sem, r=(), w=()):
        self._wait(q, self._deps(r, w))
        h = self.eng[q][0]
        ins = h.dma_start(out=out, in_=in_)
        sem.count += 16
        ins.then_inc(sem.h, 16)
        tok = (sem.h, sem.count)
        for b in r:
            b.readers.append(tok)
        for b in w:
            b.last_w = tok
            b.readers = []
        return tok


ALL_STAGES = ("ada", "s1", "s2", "s3", "s4", "s5", "s6", "s7", "s8", "s9")


def build_nc(ntok, ne=NE, stages=ALL_STAGES):
    nc = bass.Bass("TRN2", target_bir_lowering=False)
    NT = ntok // T
    NE = ne
    shapes = {}
    tiny = set()
    if "ada" not in stages:
        tiny.add("w_ada")
    if not any(x_ in stages for x_ in ("s2", "s3", "s4", "s5")):
        tiny.add("w_in")
    if "s2" not in stages:
        tiny.add("w_pool")
    if "s5" not in stages:
        tiny.update(("w_bp", "w_bs"))
    if "s6" not in stages:
        tiny.add("w_out")
    if "s8" not in stages:
        tiny.update(("w_eg", "w_eu", "w_ed"))

    def din(name, shape, dt=F32):
        shape = list(shape)
        if name in tiny:
            shape[0] = 128
        shapes[name] = tuple(shape)
        return nc.dram_tensor(name, shape, dt, kind="ExternalInput").ap()

    x = din("x", [ntok, D])
    c_in = din("c", [D])
    w_ada = din("w_ada", [D, 6 * D])
    b_ada = din("b_ada", [6 * D])
    w_in = din("w_in", [D, 14336])
    w_pool = din("w_pool", [2048, 512])
    b_pool = din("b_pool", [2048])
    pool_scale = din("pool_scale", [2048])
    ln_g = din("sgu_ln_g", [2048])
    ln_b = din("sgu_ln_b", [2048])
    w_spT = din("w_spT", [16, 128, 128])
    b_sp = din("b_sp", [1, 2048])
    w_bp = din("w_bp", [2048, D])
    w_bs = din("w_bs", [2048, D])
    w_out = din("w_out", [D, D])
    w_router = din("w_router", [D, 64])
    r_bias = din("r_bias", [1, 64])
    w_eg = din("w_eg", [NE * D, 512])
    w_eu = din("w_eu", [NE * D, 512])
    w_ed = din("w_ed", [NE * 512, D])
    fgain_in = din("final_gain", [D])
    ident_in = din("ident", [128, 128])
    tril_in = din("tril", [128, 128])
    corr_in = din("corr0", [128, 64])
    xh_in = din("x_halo", [16, D])
    hs_in = din("hscale", [128, 1])
    out = nc.dram_tensor("out", [ntok, D], F32, kind="ExternalOutput").ap()
    gdT = nc.dram_tensor("gdT", [65, T], F32).ap()

    k = Ctx(nc)

    def sb(name, shape, dt=F32):
        return nc.alloc_sbuf_tensor(name, list(shape), dt)

    ident_f = sb("ident_f", [128, 128]); B_const = Buf("const")
    ones_f = sb("ones_f", [128, 128])
    tril_f = sb("tril_f", [128, 128])
    corr0 = sb("corr0s", [128, 4, 16])
    ones_rb = sb("ones_rb", [1, 128], BF16)
    bsp_b = sb("bsp_b", [1, 2048], BF16)
    c_sb = sb("c_sb", [128, KC])
    condb = sb("condb", [128, KC], BF16)
    mod = sb("mod", [128, 192])
    bada = sb("bada", [128, 192])
    sc1p = sb("sc1p", [128, KC])
    sc2p = sb("sc2p", [128, KC])
    wsT = sb("wsT", [128, 16, 128], BF16)
    lngb = sb("lngb", [128, 2048], BF16)
    lnbb = sb("lnbb", [128, 2048], BF16)
    bpool = sb("bpool", [128, 16])
    pscale = sb("pscale", [128, 16])
    bps = sb("bps", [128, 16])
    rbias = sb("rbias", [128, 64])
    wr_sb = sb("wr_sb", [128, KC, 64])
    fgain = sb("fgain", [128, KC])
    hscale = sb("hscale_s", [128, 1])
    carry = sb("carry", [128, 16, 16]); B_carry = Buf("carry")

    xT = sb("xT", [128, KC, T]); B_xT = Buf("xT")
    hT = sb("hT", [128, KC, T], BF16); B_hT = Buf("hT")
    lntmp = xT[:, 16:24, :].rearrange("p k t -> p (k t)")
    bsp_f = xT[0:1, 24:32, :].rearrange("p k t -> p (k t)")
    yaT = sb("yaT", [128, 16, T], BF16); B_yaT = Buf("yaT")
    sT = sb("sT", [128, 16, T], BF16); B_sT = Buf("sT")
    mrg = sb("mrg", [128, KC, T], BF16); B_mrg = Buf("mrg")
    vn = mrg[:, 0:16, :].rearrange("p k t -> p (k t)").rearrange("p (s f) -> p s f", s=NSUB); B_vn = B_mrg
    aA = sb("aA", [128, 4, 16 + T]); B_aA = Buf("aA")
    aB = sb("aB", [128, 4, 16 + T]); B_aB = Buf("aB")
    aC = sb("aC", [128, 4, 16 + T]); B_aC = Buf("aC")
    diffT = sb("diffT", [128, 4, T], BF16); B_diff = Buf("diff")
    rstd = sb("rstd", [128, T]); B_rstd = Buf("rstd")
    stats = sb("stats", [128, NSUB, 8, 6]); B_stats = Buf("stats")
    mv = sb("mv", [128, NSUB, 2]); B_mv = Buf("mv")
    G_sb = sb("G_sb", [128, NSUB, 65]); B_G = Buf("G")
    GT_sb = sb("GT_sb", [65, T]); B_GT = Buf("GT")
    B_gd = Buf("gd")
    NTMP = 4
    tmps = [(sb(f"tmp{i}", [128, T]), Buf(f"tmp{i}")) for i in range(NTMP)]
    tmp_i = [0]
    rt = [(sb(f"rt{i}", [128, 64]), Buf(f"rt{i}")) for i in range(6)]
    rsm = [(sb(f"rsm{i}", [128, 8]), Buf(f"rsm{i}")) for i in range(6)]
    NGB = 2
    gbts = [(sb(f"gbt{i}", [128, T]), Buf(f"gbt{i}"), Sem(nc, f"gbt{i}")) for i in range(NGB)]
    actT = [(sb(f"actT{i}", [128, 4, T], BF16), Buf(f"actT{i}")) for i in range(2)]
    stg = [(sb(f"stg{i}", [128, 512]), Buf(f"stg{i}"), Sem(nc, f"stg{i}")) for i in range(2)]
    NRING = 2
    ring = [(sb(f"ring{i}", [128, 8192], BF16), Buf(f"ring{i}"), Sem(nc, f"ring{i}")) for i in range(NRING)]
    ring_i = [0]
    psums = [(nc.alloc_psum_tensor(f"ps{i}", [128, 512], F32), Buf(f"ps{i}")) for i in range(8)]
    ps_i = [0]
    s_const = Sem(nc, "const")
    s_gd = Sem(nc, "gd")

    def psum():
        p = psums[ps_i[0] % 8]
        ps_i[0] += 1
        return p

    def tmp():
        t_ = tmps[tmp_i[0] % NTMP]
        tmp_i[0] += 1
        return t_

    V, S, P, G = nc.vector, nc.scalar, nc.tensor, nc.gpsimd
    print('SBUF remaining', nc.sbuf_bytes_remaining)

    with nc.allow_non_contiguous_dma(reason="small const loads"):
        k.dma("sp", ident_f[:, :], ident_in[:, :], s_const, w=[B_const])
        k.dma("sp", tril_f[:, :], tril_in[:, :], s_const, w=[B_const])
        k.dma("sp", corr0[:, :, :], corr_in.rearrange("p (g t) -> p g t", g=4), s_const, w=[B_const])
        k.dma("sp", bsp_f, b_sp[:, :], s_const, w=[B_const, B_xT])
        k.dma("sp", c_sb[:, :], c_in.rearrange("(k p) -> p k", p=128), s_const, w=[B_const])
        k.dma("sp", bada[:, :], b_ada.rearrange("(j p) -> p j", p=128), s_const, w=[B_const])
        k.dma("sp", bpool[:, :], b_pool.rearrange("(j p) -> p j", p=128), s_const, w=[B_const])
        k.dma("sp", pscale[:, :], pool_scale.rearrange("(j p) -> p j", p=128), s_const, w=[B_const])
        k.dma("sp", fgain[:, :], fgain_in.rearrange("(j p) -> p j", p=128), s_const, w=[B_const])
        k.dma("sp", rbias[:, :], r_bias.partition_broadcast(128), s_const, w=[B_const])
        k.dma("sp", hscale[:, :], hs_in[:, :], s_const, w=[B_const])
        k.dma("sp", wr_sb[:, :, :], w_router.rearrange("(k p) e -> p k e", p=128), s_const, w=[B_const])
        k.dma("sp", xT[:, 0:16, 0:128], w_spT.rearrange("h s t -> s h t"), s_const, w=[B_const, B_xT])
        k.dma("sp", lntmp, ln_g.partition_broadcast(128), s_const, w=[B_const, B_xT])
    B_const.last_w = (s_const.h, s_const.count)
    B_xT.last_w = (s_const.h, s_const.count)
    cst = [B_const]
    k.op("dve", lambda: V.memset(ones_f[:, :], 1.0), w=cst)
    k.op("dve", lambda: V.memset(ones_rb[:, :], 1.0), w=cst)
    k.op("dve", lambda: V.memset(carry[:, :, :], 0.0), w=[B_carry])
    k.op("dve", lambda: V.memset(G_sb[:, :, :], 1.0), w=[B_G])
    k.op("dve", lambda: V.tensor_copy(out=bsp_b[:, :], in_=bsp_f), r=cst + [B_xT], w=cst)
    k.op("dve", lambda: V.tensor_copy(out=lngb[:, :], in_=lntmp), r=cst + [B_xT], w=cst)
    with nc.allow_non_contiguous_dma(reason="small const loads"):
        k.dma("sp", lntmp, ln_b.partition_broadcast(128), s_const, r=cst, w=cst + [B_xT])
    k.op("dve", lambda: V.tensor_copy(out=lnbb[:, :], in_=lntmp), r=cst + [B_xT], w=cst)
    for h_ in range(16):
        k.op("dve", lambda h_=h_: V.tensor_tensor(out=wsT[:, h_, :], in0=xT[:, h_, 0:128], in1=tril_f[:, :],
                                                  op=ALU.mult), r=cst + [B_xT], w=cst)
    k.op("dve", lambda: V.tensor_tensor(out=bps[:, :], in0=bpool[:, :], in1=pscale[:, :], op=ALU.mult), r=cst, w=cst)
    k.op("act", lambda: S.activation(out=condb[:, :], in_=c_sb[:, :], func=AF.Silu), r=cst, w=cst)

    NSLAB_MAX = 700
    WCN = 120
    wcaches = [nc.dram_tensor(f"wcache{i}", [WCN, 128, 8192], BF16).ap() for i in range((NSLAB_MAX + WCN - 1) // WCN)]
    B_wc = [Buf(f"wc{i}") for i in range(NSLAB_MAX)]
    st_sems = [Sem(nc, f"wst{i}") for i in range(NRING)]
    cur = {"it": None, "ctr": 0}

    def load_slab(w2d, row0, kc, col0, ncols):
        slot = ring_i[0] % NRING
        sl = ring[slot]
        ring_i[0] += 1
        t_, b_, s_ = sl
        n = kc * ncols
        view = t_[:, 0:n].rearrange("p (k n) -> p k n", k=kc)
        if cur["it"] is None or cur["it"] == 0:
            src = w2d[row0:row0 + kc * 128, col0:col0 + ncols].rearrange("(k p) n -> p k n", p=128)
            k.dma("pool", view, src, s_, w=[b_])
            if cur["it"] == 0:
                cid = cur["ctr"]; cur["ctr"] += 1
                k.dma("sp", wcaches[cid // WCN][cid % WCN, :, 0:n], t_[:, 0:n], st_sems[slot], r=[b_], w=[B_wc[cid]])
        else:
            cid = cur["ctr"]; cur["ctr"] += 1
            k.dma("pool", t_[:, 0:n], wcaches[cid // WCN][cid % WCN, :, 0:n], s_, r=[B_wc[cid]], w=[b_])
        return view, b_

    def stream(slabs, body):
        loaded = []
        nxt = 0
        depth = NRING - 1
        while nxt < min(depth, len(slabs)):
            loaded.append(load_slab(*slabs[nxt][:5])); nxt += 1
        for i, sl in enumerate(slabs):
            view, b_ = loaded[i]
            body(view, b_, sl[5])
            if nxt < len(slabs):
                loaded.append(load_slab(*slabs[nxt][:5])); nxt += 1

    def mm_fm(w2d, row0, kc, col0, ncols_total, rhs_fn, rhs_bufs, evac, nfree=T):
        slabs = [(w2d, row0, kc, col0 + j * SLABW, SLABW, j) for j in range(ncols_total // SLABW)]

        def body(view, b_, j):
            for ml in range(SLABW // 128):
                pt, pb = psum()
                for kk in range(kc):
                    k.op("pe", lambda kk=kk: P.matmul(pt[:, 0:nfree], lhsT=view[:, kk, ml * 128:(ml + 1) * 128],
                                                      rhs=rhs_fn(kk), start=(kk == 0), stop=(kk == kc - 1)),
                         r=[b_] + rhs_bufs, w=[pb], inc=(kk == kc - 1))
                evac(j * (SLABW // 128) + ml, pt, pb)
        stream(slabs, body)

    if "ada" in stages:
        modps, modpb = psum()

        def ada_body(view, b_, j):
            for ml in range(2):
                col = j * 2 + ml
                for kk in range(KC):
                    k.op("pe", lambda kk=kk: P.matmul(modps[:, col:col + 1], lhsT=view[:, kk, ml * 128:(ml + 1) * 128],
                                                      rhs=condb[:, kk:kk + 1], start=(kk == 0), stop=(kk == KC - 1)),
                         r=[b_] + cst, w=[modpb], inc=(kk == KC - 1))
        stream([(w_ada, 0, KC, j * SLABW, SLABW, j) for j in range(6 * D // SLABW)], ada_body)
        k.op("dve", lambda: V.tensor_tensor(out=mod[:, :], in0=modps[:, 0:192], in1=bada[:, :], op=ALU.add),
             r=[modpb] + cst, w=cst)
        k.op("dve", lambda: V.tensor_scalar(out=sc1p[:, :], in0=mod[:, 32:64], scalar1=1.0, scalar2=None, op0=ALU.add),
             r=cst, w=cst)
        k.op("dve", lambda: V.tensor_scalar(out=sc2p[:, :], in0=mod[:, 128:160], scalar1=1.0, scalar2=None, op0=ALU.add),
             r=cst, w=cst)
    sh1 = mod[:, 0:32]; g1 = mod[:, 64:96]; sh2 = mod[:, 96:128]; g2 = mod[:, 160:192]

    def rms_stats():
        pt, pb = psum()
        for kk in range(KC):
            tt, tb = tmp()
            k.op("act", lambda: S.activation(out=tt[:, :], in_=xT[:, kk, :], func=AF.Square), r=[B_xT], w=[tb])
            k.op("pe", lambda: P.matmul(pt[:, 0:T], lhsT=ones_f[:, :], rhs=tt[:, :], start=(kk == 0),
                                        stop=(kk == KC - 1)), r=[tb] + cst, w=[pb], inc=True)
        k.op("dve", lambda: V.tensor_scalar(out=rstd[:, :], in0=pt[:, 0:T], scalar1=1.0 / D, scalar2=EPS,
                                            op0=ALU.mult, op1=ALU.add), r=[pb], w=[B_rstd])
        k.op("act", lambda: S.activation(out=rstd[:, :], in_=rstd[:, :], func=AF.Sqrt), r=[B_rstd], w=[B_rstd])
        k.op("dve", lambda: V.reciprocal(out=rstd[:, :], in_=rstd[:, :]), r=[B_rstd], w=[B_rstd])

    if "s2" in stages:
        for cpc in range(8):
            st, stb, sts = stg[cpc % 2]
            k.dma("sp", st[0:16, :], xh_in[:, cpc * 512:(cpc + 1) * 512], sts, w=[stb])
            pt, pb = psum()
            for j in range(4):
                k.op("pe", lambda j=j: P.transpose(out=pt[:, j * 16:(j + 1) * 16], in_=st[0:16, j * 128:(j + 1) * 128],
                                                   identity=ident_f[0:16, 0:16]), r=[stb] + cst, w=[pb], inc=(j == 3))
            k0 = cpc * 4
            k.op("act", lambda: S.copy(out=xT[:, k0:k0 + 4, 0:16], in_=pt[:, 0:64].rearrange("p (a b) -> p a b", a=4)),
                 r=[pb], w=[B_xT])
        rms_stats()
        for kk in range(KC):
            tt, tb = tmp()
            k.op("dve", lambda: V.tensor_tensor(out=tt[:, :], in0=xT[:, kk, :], in1=rstd[:, :], op=ALU.mult),
                 r=[B_xT, B_rstd], w=[tb])
            k.op("act", lambda: S.activation(out=hT[:, kk, :], in_=tt[:, :], func=AF.Identity,
                                             bias=sh1[:, kk:kk + 1], scale=sc1p[:, kk:kk + 1]),
                 r=[tb] + cst, w=[B_hT])

        def evac_halo(m, pt, pb):
            k.op("dve", lambda: V.tensor_scalar(out=carry[:, m, :], in0=pt[:, 0:16], scalar1=hscale[:, 0:1], scalar2=None,
                                                op0=ALU.mult), r=[pb] + cst, w=[B_carry])
        mm_fm(w_in, 0, KC, 0, 2048, lambda kk: hT[:, kk, 0:16], [B_hT], evac_halo, nfree=16)

    for it in range(NT):
        t0 = it * T
        cur["it"] = it
        cur["ctr"] = 0
        if "s1" in stages:
            for blk in range(NSUB):
                for cpc in range(8):
                    st, stb, sts = stg[(blk * 8 + cpc) % 2]
                    k.dma("sp", st[:, :], x[t0 + blk * 128:t0 + (blk + 1) * 128, cpc * 512:(cpc + 1) * 512], sts, w=[stb])
                    pt, pb = psum()
                    for j in range(4):
                        k.op("pe", lambda j=j: P.transpose(out=pt[:, j * 128:(j + 1) * 128],
                                                           in_=st[:, j * 128:(j + 1) * 128],
                                                           identity=ident_f[:, :]),
                             r=[stb] + cst, w=[pb], inc=(j == 3))
                    k0 = cpc * 4
                    k.op("act", lambda: S.copy(out=xT[:, k0:k0 + 4, blk * 128:(blk + 1) * 128],
                                               in_=pt[:, 0:512].rearrange("p (a b) -> p a b", a=4)),
                         r=[pb], w=[B_xT])
            rms_stats()
            for kk in range(KC):
                tt, tb = tmp()
                k.op("dve", lambda: V.tensor_tensor(out=tt[:, :], in0=xT[:, kk, :], in1=rstd[:, :], op=ALU.mult),
                     r=[B_xT, B_rstd], w=[tb])
                k.op("act", lambda: S.activation(out=hT[:, kk, :], in_=tt[:, :], func=AF.Identity,
                                                 bias=sh1[:, kk:kk + 1], scale=sc1p[:, kk:kk + 1]),
                     r=[tb] + cst, w=[B_hT])

        if "s2" in stages:
            for g in range(4):
                def evac_a(m, pt, pb):
                    k.op("act", lambda: S.copy(out=aA[:, m, 16:16 + T], in_=pt[:, 0:T]), r=[pb], w=[B_aA])
                mm_fm(w_in, 0, KC, g * 512, 512, lambda kk: hT[:, kk, :], [B_hT], evac_a)
                k.op("dve", lambda: V.tensor_copy(out=aA[:, :, 0:16], in_=carry[:, g * 4:(g + 1) * 4, :]),
                     r=[B_carry], w=[B_aA])
                L = 16 + T
                k.op("dve", lambda: V.tensor_tensor(out=aB[:, :, 1:L], in0=aA[:, :, 1:L], in1=aA[:, :, 0:L - 1], op=ALU.add),
                     r=[B_aA], w=[B_aB])
                src_t, src_b = aB, B_aB
                if g >= 1:
                    k.op("dve", lambda: V.tensor_tensor(out=aC[:, :, 3:L], in0=aB[:, :, 3:L], in1=aB[:, :, 1:L - 2], op=ALU.add),
                         r=[B_aB], w=[B_aC])
                    src_t, src_b = aC, B_aC
                if g >= 2:
                    k.op("dve", lambda: V.tensor_tensor(out=aB[:, :, 7:L], in0=aC[:, :, 7:L], in1=aC[:, :, 3:L - 4], op=ALU.add),
                         r=[B_aC], w=[B_aB])
                    src_t, src_b = aB, B_aB
                if g >= 3:
                    k.op("dve", lambda: V.tensor_tensor(out=aC[:, :, 15:L], in0=aB[:, :, 15:L], in1=aB[:, :, 7:L - 8], op=ALU.add),
                         r=[B_aB], w=[B_aC])
                    src_t, src_b = aC, B_aC
                wdw = float(2 ** (g + 1))
                if it == 0:
                    k.op("dve", lambda: V.tensor_tensor(out=src_t[:, :, 16:32], in0=src_t[:, :, 16:32],
                                                        in1=corr0[:, g:g + 1, :].to_broadcast([128, 4, 16]), op=ALU.mult),
                         r=[src_b] + cst, w=[src_b])
                k.op("dve", lambda: V.scalar_tensor_tensor(out=diffT[:, :, :], in0=src_t[:, :, 16:L], scalar=1.0 / wdw,
                                                           in1=aA[:, :, 16:L], op0=ALU.mult, op1=ALU.subtract),
                     r=[src_b, B_aA], w=[B_diff])
                k.op("dve", lambda: V.tensor_copy(out=carry[:, g * 4:(g + 1) * 4, :], in_=aA[:, :, T:T + 16]),
                     r=[B_aA], w=[B_carry])

                def evac_ya(m, pt, pb):
                    mm = g * 4 + m
                    k.op("dve", lambda: V.tensor_scalar(out=yaT[:, mm, :], in0=pt[:, 0:T], scalar1=pscale[:, mm:mm + 1],
                                                        scalar2=bps[:, mm:mm + 1], op0=ALU.mult, op1=ALU.add),
                         r=[pb] + cst, w=[B_yaT])
                mm_fm(w_pool, g * 512, 4, 0, 512, lambda kk: diffT[:, kk, :], [B_diff], evac_ya)

        if "s3" in stages:
            def v_body(view, b_, j):
                for blk in range(NSUB):
                    pt, pb = psum()
                    for kk in range(KC):
                        k.op("pe", lambda kk=kk: P.matmul(pt[:, 0:SLABW], lhsT=hT[:, kk, blk * 128:(blk + 1) * 128],
                                                          rhs=view[:, kk, :], start=(kk == 0), stop=(kk == KC - 1)),
                             r=[b_, B_hT], w=[pb], inc=(kk == KC - 1))
                    if DBG < 2:
                        continue
                    qt, qb = tmp()
                    k.op("act", lambda: S.activation(out=qt[:, :], in_=pt[:, 0:SLABW], func=AF.Square), r=[pb], w=[qb])
                    k.op("dve", lambda: V.tensor_reduce(out=stats[:, blk, j, 0:1], in_=pt[:, 0:SLABW], axis=AX.X, op=ALU.add),
                         r=[pb, qb], w=[B_stats])
                    k.op("dve", lambda: V.tensor_reduce(out=stats[:, blk, j, 1:2], in_=qt[:, :], axis=AX.X, op=ALU.add),
                         r=[qb], w=[B_stats])
                    k.op("act", lambda: S.copy(out=vn[:, blk, j * SLABW:(j + 1) * SLABW], in_=pt[:, 0:SLABW]),
                         r=[pb], w=[B_vn])
            stream([(w_in, 0, KC, 4096 + j * SLABW, SLABW, j) for j in range(8)], v_body)
            if DBG >= 3:
                k.op("dve", lambda: V.tensor_reduce(out=mv[:, :, 0], in_=stats[:, :, :, 0], axis=AX.X, op=ALU.add),
                     r=[B_stats], w=[B_mv])
                k.op("dve", lambda: V.tensor_reduce(out=mv[:, :, 1], in_=stats[:, :, :, 1], axis=AX.X, op=ALU.add),
                     r=[B_stats], w=[B_mv])
                k.op("dve", lambda: V.tensor_scalar(out=mv[:, :, :], in0=mv[:, :, :], scalar1=1.0 / 2048.0, scalar2=None,
                                                    op0=ALU.mult), r=[B_mv], w=[B_mv])
                k.op("dve", lambda: V.tensor_tensor(out=stats[:, :, 0, 2], in0=mv[:, :, 0], in1=mv[:, :, 0], op=ALU.mult),
                     r=[B_mv], w=[B_stats])
                k.op("dve", lambda: V.tensor_tensor(out=mv[:, :, 1], in0=mv[:, :, 1], in1=stats[:, :, 0, 2], op=ALU.subtract),
                     r=[B_mv, B_stats], w=[B_mv])
                k.op("dve", lambda: V.tensor_scalar(out=mv[:, :, 1], in0=mv[:, :, 1], scalar1=EPS, scalar2=None,
                                                    op0=ALU.add), r=[B_mv], w=[B_mv])
                k.op("act", lambda: S.activation(out=mv[:, :, 1], in_=mv[:, :, 1], func=AF.Sqrt), r=[B_mv], w=[B_mv])
                k.op("dve", lambda: V.reciprocal(out=mv[:, :, 1], in_=mv[:, :, 1]), r=[B_mv], w=[B_mv])
            for blk in range(NSUB):
                if DBG < 4:
                    continue
                k.op("dve", lambda: V.tensor_scalar(out=vn[:, blk, :], in0=vn[:, blk, :], scalar1=mv[:, blk, 0:1],
                                                    scalar2=mv[:, blk, 1:2], op0=ALU.subtract, op1=ALU.mult),
                     r=[B_mv, B_vn], w=[B_vn])
                k.op("dve", lambda: V.tensor_tensor(out=vn[:, blk, :], in0=vn[:, blk, :], in1=lngb[:, :], op=ALU.mult),
                     r=[B_vn] + cst, w=[B_vn])
                k.op("dve", lambda: V.tensor_tensor(out=vn[:, blk, :], in0=vn[:, blk, :], in1=lnbb[:, :], op=ALU.add),
                     r=[B_vn] + cst, w=[B_vn])

        if "s4" in stages:
            def evac_u(hh, pt, pb):
                ut, ub = tmp()
                k.op("act", lambda: S.copy(out=ut[:, :], in_=pt[:, 0:T]), r=[pb], w=[ub])
                zt, zb = psum()
                for blk in range(NSUB):
                    k.op("pe", lambda: P.matmul(zt[:, blk * 128:(blk + 1) * 128], lhsT=vn[:, blk, hh * 128:(hh + 1) * 128],
                                                rhs=wsT[:, hh, :], start=True, stop=False), r=[B_vn] + cst, w=[zb], inc=False)
                    k.op("pe", lambda: P.matmul(zt[:, blk * 128:(blk + 1) * 128], lhsT=ones_rb[0:1, :],
                                                rhs=bsp_b[0:1, hh * 128:(hh + 1) * 128], start=False, stop=True),
                         r=cst, w=[zb], inc=(blk == NSUB - 1))
                k.op("dve", lambda: V.tensor_tensor(out=sT[:, hh, :], in0=zt[:, 0:T], in1=ut[:, :], op=ALU.mult),
                     r=[zb, ub], w=[B_sT])
            mm_fm(w_in, 0, KC, 2048, 2048, lambda kk: hT[:, kk, :], [B_hT], evac_u)

        if "s5" in stages:
            for mp in range(16):
                c0 = mp * SLABW
                slabs = [(w_in, 0, KC, 6144 + c0, SLABW, 0), (w_bp, 0, 16, c0, SLABW, 1),
                         (w_in, 0, KC, 10240 + c0, SLABW, 2), (w_bs, 0, 16, c0, SLABW, 3)]
                res = {}

                def s5_body(view, b_, kind):
                    kc_ = KC if kind in (0, 2) else 16
                    src = {0: (hT, B_hT), 1: (yaT, B_yaT), 2: (hT, B_hT), 3: (sT, B_sT)}[kind]
                    for ml in range(2):
                        pt, pb = psum()
                        for kk in range(kc_):
                            k.op("pe", lambda kk=kk: P.matmul(pt[:, 0:T], lhsT=view[:, kk, ml * 128:(ml + 1) * 128],
                                                              rhs=src[0][:, kk, :], start=(kk == 0), stop=(kk == kc_ - 1)),
                                 r=[b_, src[1]], w=[pb], inc=(kk == kc_ - 1))
                        if kind in (0, 2):
                            st_, sb_ = tmp()
                            k.op("act", lambda: S.activation(out=st_[:, :], in_=pt[:, 0:T], func=AF.Sigmoid), r=[pb], w=[sb_])
                            res[(kind, ml)] = (st_, sb_)
                        else:
                            gt_, gb_ = res[(kind - 1, ml)]
                            m = mp * 2 + ml
                            if kind == 1:
                                k.op("dve", lambda: V.tensor_tensor(out=gt_[:, :], in0=gt_[:, :], in1=pt[:, 0:T], op=ALU.mult),
                                     r=[pb, gb_], w=[gb_])
                                res[("a", ml)] = (gt_, gb_)
                            else:
                                at_, ab_ = res[("a", ml)]
                                k.op("dve", lambda: V.tensor_tensor(out=gt_[:, :], in0=gt_[:, :], in1=pt[:, 0:T], op=ALU.mult),
                                     r=[pb, gb_], w=[gb_])
                                k.op("dve", lambda: V.tensor_tensor(out=mrg[:, m, :], in0=gt_[:, :], in1=at_[:, :], op=ALU.add),
                                     r=[gb_, ab_], w=[B_mrg])
                stream(slabs, s5_body)

        if "s6" in stages:
            def evac_x1(m, pt, pb):
                k.op("dve", lambda: V.scalar_tensor_tensor(out=xT[:, m, :], in0=pt[:, 0:T], scalar=g1[:, m:m + 1],
                                                           in1=xT[:, m, :], op0=ALU.mult, op1=ALU.add),
                     r=[pb, B_xT] + cst, w=[B_xT])
            mm_fm(w_out, 0, KC, 0, D, lambda kk: mrg[:, kk, :], [B_mrg], evac_x1)

        if "s7" in stages:
            rms_stats()
            lgs = [psum() for _ in range(NSUB)]
            for kk in range(KC):
                tt, tb = tmp()
                k.op("dve", lambda: V.tensor_tensor(out=tt[:, :], in0=xT[:, kk, :], in1=rstd[:, :], op=ALU.mult),
                     r=[B_xT, B_rstd], w=[tb])
                k.op("act", lambda: S.activation(out=tt[:, :], in_=tt[:, :], func=AF.Identity,
                                                 bias=sh2[:, kk:kk + 1], scale=sc2p[:, kk:kk + 1]), r=[tb] + cst, w=[tb])
                k.op("dve", lambda: V.tensor_copy(out=hT[:, kk, :], in_=tt[:, :]), r=[tb], w=[B_hT])
                for blk in range(NSUB):
                    k.op("pe", lambda: P.matmul(lgs[blk][0][:, 0:64], lhsT=tt[:, blk * 128:(blk + 1) * 128],
                                                rhs=wr_sb[:, kk, :], start=(kk == 0), stop=(kk == KC - 1)),
                         r=[tb] + cst, w=[lgs[blk][1]], inc=True)
            for blk in range(NSUB):
                (sc_, scb), (bi_, bib), (t3_, t3b), (ms_, msb), (sel_, selb), (w_, wb) = rt
                (m1_, m1b), (m2_, m2b), (gs_, gsb), (g8_, g8b), (gm_, gmb), (e8_, e8b) = rsm
                k.op("act", lambda: S.activation(out=sc_[:, :], in_=lgs[blk][0][:, 0:64], func=AF.Sigmoid),
                     r=[lgs[blk][1]], w=[scb])
                k.op("dve", lambda: V.tensor_tensor(out=bi_[:, :], in0=sc_[:, :], in1=rbias[:, :], op=ALU.add),
                     r=[scb] + cst, w=[bib])
                bi3 = bi_[:, :].rearrange("p (g e) -> p g e", g=8)
                t33 = t3_[:, :].rearrange("p (g e) -> p g e", g=8)
                k.op("dve", lambda: V.tensor_reduce(out=m1_[:, :], in_=bi3, axis=AX.X, op=ALU.max), r=[bib], w=[m1b])
                k.op("dve", lambda: V.tensor_tensor(out=t33, in0=bi3, in1=m1_[:, :].unsqueeze(2).to_broadcast([128, 8, 8]),
                                                    op=ALU.is_equal), r=[bib, m1b], w=[t3b])
                k.op("dve", lambda: V.scalar_tensor_tensor(out=t3_[:, :], in0=t3_[:, :], scalar=-BIG, in1=bi_[:, :],
                                                           op0=ALU.mult, op1=ALU.add), r=[t3b, bib], w=[t3b])
                k.op("dve", lambda: V.tensor_reduce(out=m2_[:, :], in_=t33, axis=AX.X, op=ALU.max), r=[t3b], w=[m2b])
                k.op("dve", lambda: V.tensor_tensor(out=gs_[:, :], in0=m1_[:, :], in1=m2_[:, :], op=ALU.add),
                     r=[m1b, m2b], w=[gsb])
                k.op("dve", lambda: V.max(out=g8_[:, :], in_=gs_[:, :]), r=[gsb], w=[g8b])
                k.op("dve", lambda: V.tensor_scalar(out=gm_[:, :], in0=gs_[:, :], scalar1=g8_[:, 3:4], scalar2=None,
                                                    op0=ALU.is_ge), r=[gsb, g8b], w=[gmb])
                k.op("dve", lambda: V.tensor_scalar(out=gm_[:, :], in0=gm_[:, :], scalar1=-1.0, scalar2=BIG,
                                                    op0=ALU.add, op1=ALU.mult), r=[gmb], w=[gmb])
                ms3 = ms_[:, :].rearrange("p (g e) -> p g e", g=8)
                k.op("dve", lambda: V.tensor_tensor(out=ms3, in0=bi3, in1=gm_[:, :].unsqueeze(2).to_broadcast([128, 8, 8]),
                                                    op=ALU.add), r=[bib, gmb], w=[msb])
                k.op("dve", lambda: V.max(out=e8_[:, :], in_=ms_[:, :]), r=[msb], w=[e8b])
                k.op("dve", lambda: V.tensor_scalar(out=sel_[:, :], in0=ms_[:, :], scalar1=e8_[:, 7:8], scalar2=None,
                                                    op0=ALU.is_ge), r=[msb, e8b], w=[selb])
                k.op("dve", lambda: V.tensor_tensor(out=w_[:, :], in0=sc_[:, :], in1=sel_[:, :], op=ALU.mult),
                     r=[scb, selb], w=[wb])
                k.op("dve", lambda: V.tensor_reduce(out=m1_[:, 0:1], in_=w_[:, :], axis=AX.X, op=ALU.add), r=[wb], w=[m1b])
                k.op("dve", lambda: V.reciprocal(out=m1_[:, 0:1], in_=m1_[:, 0:1]), r=[m1b], w=[m1b])
                k.op("dve", lambda: V.tensor_scalar(out=G_sb[:, blk, 0:64], in0=w_[:, :], scalar1=m1_[:, 0:1], scalar2=2.5,
                                                    op0=ALU.mult, op1=ALU.mult), r=[wb, m1b], w=[B_G])
                pt, pb = psum()
                k.op("pe", lambda: P.transpose(out=pt[0:65, 0:128], in_=G_sb[:, blk, :], identity=ident_f[:, :]),
                     r=[B_G] + cst, w=[pb])
                k.op("act", lambda: S.copy(out=GT_sb[:, blk * 128:(blk + 1) * 128], in_=pt[0:65, 0:128]), r=[pb], w=[B_GT])
            k.dma("sp", gdT[:, :], GT_sb[:, :], s_gd, r=[B_GT], w=[B_gd])

        if "s8" in stages:
            slabs = []
            for e in range(NE):
                for fp in range(2):
                    slabs.append((w_eg, e * D, KC, fp * SLABW, SLABW, (e, "g", fp)))
                    slabs.append((w_eu, e * D, KC, fp * SLABW, SLABW, (e, "u", fp)))
                for dh in range(2):
                    slabs.append((w_ed, e * 512, 4, dh * 2048, 2048, (e, "d", dh)))
            st8 = {}

            def moe_body(view, b_, info):
                e, kind, idx = info
                at_, ab_ = actT[e % 2]
                if kind == "g":
                    st8["g"] = []
                    for fl in range(2):
                        pt, pb = psum()
                        for kk in range(KC):
                            k.op("pe", lambda kk=kk: P.matmul(pt[:, 0:T], lhsT=view[:, kk, fl * 128:(fl + 1) * 128],
                                                              rhs=hT[:, kk, :], start=(kk == 0), stop=(kk == KC - 1)),
                                 r=[b_, B_hT], w=[pb], inc=(kk == KC - 1))
                        gt_, gb_ = tmp()
                        k.op("act", lambda: S.activation(out=gt_[:, :], in_=pt[:, 0:T], func=AF.Silu), r=[pb], w=[gb_])
                        st8["g"].append((gt_, gb_))
                    if idx == 0:
                        gbt, gbb, gbs = gbts[e % NGB]
                        with nc.allow_non_contiguous_dma(reason="bcast"):
                            k.dma("sp", gbt[:, :], gdT[(e if e < NE - 1 else 64):(e if e < NE - 1 else 64) + 1, :].partition_broadcast(128), gbs, r=[B_gd], w=[gbb])
                elif kind == "u":
                    gbt, gbb, gbs = gbts[e % NGB]
                    for fl in range(2):
                        pt, pb = psum()
                        for kk in range(KC):
                            k.op("pe", lambda kk=kk: P.matmul(pt[:, 0:T], lhsT=view[:, kk, fl * 128:(fl + 1) * 128],
                                                              rhs=hT[:, kk, :], start=(kk == 0), stop=(kk == KC - 1)),
                                 r=[b_, B_hT], w=[pb], inc=(kk == KC - 1))
                        gt_, gb_ = st8["g"][fl]
                        k.op("dve", lambda: V.tensor_tensor(out=gt_[:, :], in0=gt_[:, :], in1=pt[:, 0:T], op=ALU.mult),
                             r=[pb, gb_], w=[gb_])
                        k.op("dve", lambda: V.tensor_tensor(out=at_[:, idx * 2 + fl, :], in0=gt_[:, :], in1=gbt[:, :],
                                                            op=ALU.mult), r=[gb_, gbb], w=[ab_])
                else:
                    for ml in range(16):
                        m = idx * 16 + ml
                        pt, pb = psum()
                        for kk in range(4):
                            k.op("pe", lambda kk=kk: P.matmul(pt[:, 0:T], lhsT=view[:, kk, ml * 128:(ml + 1) * 128],
                                                              rhs=at_[:, kk, :], start=(kk == 0), stop=(kk == 3)),
                                 r=[b_, ab_], w=[pb], inc=(kk == 3))
                        k.op("dve", lambda: V.scalar_tensor_tensor(out=xT[:, m, :], in0=pt[:, 0:T], scalar=g2[:, m:m + 1],
                                                                   in1=xT[:, m, :], op0=ALU.mult, op1=ALU.add),
                             r=[pb, B_xT] + cst, w=[B_xT])
            stream(slabs, moe_body)

        if "s9" in stages:
            rms_stats()
            for blk in range(NSUB):
                for cpc in range(8):
                    st, stb, sts = stg[(blk * 8 + cpc) % 2]
                    pt, pb = psum()
                    for j in range(4):
                        kk = cpc * 4 + j
                        ot, ob = tmp()
                        k.op("dve", lambda: V.scalar_tensor_tensor(out=ot[:, 0:128], in0=xT[:, kk, blk * 128:(blk + 1) * 128],
                                                                   scalar=fgain[:, kk:kk + 1],
                                                                   in1=rstd[:, blk * 128:(blk + 1) * 128],
                                                                   op0=ALU.mult, op1=ALU.mult),
                             r=[B_xT, B_rstd] + cst, w=[ob])
                        k.op("pe", lambda: P.transpose(out=pt[:, j * 128:(j + 1) * 128], in_=ot[:, 0:128],
                                                       identity=ident_f[:, :]), r=[ob] + cst, w=[pb], inc=True)
                    k.op("act", lambda: S.copy(out=st[:, :], in_=pt[:, 0:512]), r=[pb], w=[stb])
                    k.dma("sp", out[t0 + blk * 128:t0 + (blk + 1) * 128, cpc * 512:(cpc + 1) * 512], st[:, :], sts,
                          r=[stb], w=[stb])

    for st, stb, sts in stg:
        nc.sync.wait_ge(sts.h, sts.count)
    nc._in_shapes = shapes
    return nc


_CACHE = {}


def _host_consts():
    ident = np.eye(128, dtype=np.float32)
    s = np.arange(128)[:, None]; t = np.arange(128)[None, :]
    tril = (s <= t).astype(np.float32)
    corr = np.zeros((4, 16), np.float32)
    for g, w in enumerate((2, 4, 8, 16)):
        for p in range(16):
            corr[g, p] = w / min(p + 1, w)
    corr0 = np.ascontiguousarray(np.broadcast_to(corr.reshape(1, 64), (128, 64))).astype(np.float32)
    return ident, tril, corr0


def kernel(x, c, w_ada, b_ada, w_in, w_pool, b_pool, pool_scale, sgu_ln_g, sgu_ln_b, w_spatial, b_spatial,
           w_branch_pool, w_branch_sgu, w_out, w_router, router_bias, w_exp_gate, w_exp_up, w_exp_down,
           w_sh_gate, w_sh_up, w_sh_down, final_gain, _ntok=SEQ, _ncores=NCORES, _ne=NE, _stages=ALL_STAGES, _start=0):
    f = lambda a: np.ascontiguousarray(np.asarray(a, dtype=np.float32))
    ident, tril, corr0 = _host_consts()
    shared = dict(
        w_ada=f(w_ada[0]), b_ada=f(b_ada[0]), w_in=f(w_in[0]), w_pool=f(w_pool[0]).reshape(2048, 512),
        b_pool=f(b_pool[0]).reshape(2048), pool_scale=f(pool_scale[0]), sgu_ln_g=f(sgu_ln_g[0]), sgu_ln_b=f(sgu_ln_b[0]),
        w_spT=f(np.transpose(np.asarray(w_spatial[0]), (0, 2, 1))), b_sp=f(b_spatial[0]).reshape(1, 2048),
        w_bp=f(w_branch_pool[0]), w_bs=f(w_branch_sgu[0]), w_out=f(w_out[0]), w_router=f(w_router[0]),
        r_bias=f(router_bias[0]).reshape(1, 64),
        w_eg=f(np.concatenate([np.asarray(w_exp_gate[0]).reshape(64 * D, 512)[:(_ne - 1) * D], np.asarray(w_sh_gate[0])], 0)),
        w_eu=f(np.concatenate([np.asarray(w_exp_up[0]).reshape(64 * D, 512)[:(_ne - 1) * D], np.asarray(w_sh_up[0])], 0)),
        w_ed=f(np.concatenate([np.asarray(w_exp_down[0]).reshape(64 * 512, D)[:(_ne - 1) * 512], np.asarray(w_sh_down[0])], 0)),
        final_gain=f(final_gain), ident=ident, tril=tril, corr0=corr0,
    )
    xs = np.asarray(x, dtype=np.float32)
    cs = np.asarray(c, dtype=np.float32)
    nsplit = max(1, _ncores // 4)
    per = min(_ntok, SEQ // nsplit)
    ones_corr = np.ones((128, 64), np.float32)
    in_maps = []
    for cidx in range(_ncores):
        b, half = cidx // nsplit, cidx % nsplit
        m = dict(shared)
        lo = half * per + _start
        m["x"] = np.ascontiguousarray(xs[b, lo:lo + per])
        m["c"] = np.ascontiguousarray(cs[b])
        if lo == 0:
            m["x_halo"] = np.zeros((16, D), np.float32)
            m["hscale"] = np.zeros((128, 1), np.float32)
        else:
            m["x_halo"] = np.ascontiguousarray(xs[b, lo - 16:lo])
            m["hscale"] = np.ones((128, 1), np.float32)
            m["corr0"] = ones_corr
        in_maps.append(m)
    _ntok = per
    key = (_ntok, _ne, tuple(_stages))
    if key not in _CACHE:
        _CACHE[key] = build_nc(_ntok, _ne, tuple(_stages))
    nc = _CACHE[key]
    for m in in_maps:
        for name, shp in nc._in_shapes.items():
            if m[name].shape != shp:
                m[name] = np.ascontiguousarray(m[name][:shp[0]])
    res = run_bass_kernel_spmd(nc, in_maps, core_ids=list(range(_ncores)))
    outs = [np.asarray(r["out"], dtype=np.float32) for r in res.results]
    nb = _ncores // nsplit
    return np.stack([np.concatenate(outs[b * nsplit:(b + 1) * nsplit], 0) for b in range(nb)], 0)
```
